# Optimizing a Trainium2 kernel written in Bass

```python
import jax, jax.numpy as jnp
from jax import lax
import numpy as np

D_MODEL = 1024
BATCH = 32
SEQ = 2048
DEPTH = 1

HEAD_DIM = 64
N_HEADS = D_MODEL // HEAD_DIM
N_HEADS_NA = N_HEADS // 2
N_HEADS_DIL = N_HEADS - N_HEADS_NA
GRID_W = 64
NA_ROWS_MAX = 8
NA_COLS = 16
DIL_PAIRS = ((128, 1), (512, 4), (2048, 16))
ROPE_THETA = 10000.0
N_GROUPS = 4
EXPERTS_PER_GROUP = 8
N_EXPERTS = N_GROUPS * EXPERTS_PER_GROUP
TOP_K_IN_GROUP = 2
D_EXPERT = 512
PLE_DIM = 256
EPS = 1e-6
NEG = -1e30

kernel_name = "hybrid_na_dilated_hiermoe_encoder"


def rms_norm(x, gain):
    xf = x.astype(jnp.float32)
    y = xf * lax.rsqrt(jnp.mean(xf * xf, axis=-1, keepdims=True) + EPS)
    return (y * gain.astype(jnp.float32)).astype(x.dtype)


def rope(x, pos):
    half = x.shape[-1] // 2
    inv = ROPE_THETA ** (-jnp.arange(half, dtype=jnp.float32) / half)
    ang = pos.astype(jnp.float32)[:, None] * inv[None, :]
    cos = jnp.cos(ang).astype(x.dtype)
    sin = jnp.sin(ang).astype(x.dtype)
    x1, x2 = x[..., :half], x[..., half:]
    return jnp.concatenate([x1 * cos - x2 * sin, x1 * sin + x2 * cos], axis=-1)


def neighbourhood_attention(q, k, v, rpb):
    b, h, s, dh = q.shape
    rows = s // GRID_W
    kr = min(NA_ROWS_MAX, rows)
    qg = q.reshape(b, h, rows, GRID_W, dh)
    kg = k.reshape(b, h, rows, GRID_W, dh)
    vg = v.reshape(b, h, rows, GRID_W, dh)
    row_start = jnp.clip(jnp.arange(rows) - kr // 2, 0, rows - kr)
    col_start = jnp.clip(jnp.arange(GRID_W) - NA_COLS // 2, 0, GRID_W - NA_COLS)
    col_idx = col_start[:, None] + jnp.arange(NA_COLS)[None, :]
    col_off = col_idx - jnp.arange(GRID_W)[:, None] + (NA_COLS - 1)

    def one_row(r):
        rs = row_start[r]
        q_r = lax.dynamic_index_in_dim(qg, r, axis=2, keepdims=False)
        k_rows = lax.dynamic_slice_in_dim(kg, rs, kr, axis=2)
        v_rows = lax.dynamic_slice_in_dim(vg, rs, kr, axis=2)
        k_win = k_rows[:, :, :, col_idx]
        v_win = v_rows[:, :, :, col_idx]
        row_off = rs + jnp.arange(kr) - r + (NA_ROWS_MAX - 1)
        bias = rpb[:, row_off][:, :, col_off]
        bias = bias.transpose(0, 2, 1, 3).astype(jnp.float32)
        sc = jnp.einsum('bhwd,bhrwkd->bhwrk', q_r, k_win).astype(jnp.float32) + bias[None]
        pr = jax.nn.softmax(sc.reshape(b, h, GRID_W, kr * NA_COLS), axis=-1)
        pr = pr.astype(v.dtype).reshape(b, h, GRID_W, kr, NA_COLS)
        return jnp.einsum('bhwrk,bhrwkd->bhwd', pr, v_win)

    out = lax.map(one_row, jnp.arange(rows))
    return out.transpose(1, 2, 0, 3, 4).reshape(b, h, s, dh)


def dilated_window_attention(q, k, v, window, dilation):
    b, h, s, dh = q.shape
    radius = window // (2 * dilation)
    blk = radius
    unit = dilation * blk
    s_pad = -(-s // unit) * unit
    pad = s_pad - s
    l = s_pad // dilation
    nb = l // blk

    def split(t):
        t = jnp.pad(t, ((0, 0), (0, 0), (0, pad), (0, 0)))
        t = t.reshape(b, h, l, dilation, dh).transpose(0, 1, 3, 2, 4)
        return t.reshape(b, h, dilation, nb, blk, dh)

    def band(t):
        tp = jnp.pad(t, ((0, 0), (0, 0), (0, 0), (1, 1), (0, 0), (0, 0)))
        return jnp.concatenate([tp[:, :, :, 0:nb], tp[:, :, :, 1:nb + 1], tp[:, :, :, 2:nb + 2]], axis=4)

    qs = split(q)
    kb = band(split(k))
    vb = band(split(v))
    sc = jnp.einsum('bhrnqd,bhrnkd->bhrnqk', qs, kb).astype(jnp.float32)
    lq = jnp.arange(nb)[:, None, None] * blk + jnp.arange(blk)[None, :, None]
    lk = (jnp.arange(nb)[:, None, None] - 1) * blk + jnp.arange(3 * blk)[None, None, :]
    pos_k = lk[None] * dilation + jnp.arange(dilation)[:, None, None, None]
    valid = (jnp.abs(lk - lq) <= radius)[None] & (lk >= 0)[None] & (pos_k < s)
    sc = jnp.where(valid, sc, NEG)
    m = jnp.max(sc, axis=-1, keepdims=True)
    e = jnp.exp(sc - m)
    den = jnp.sum(e, axis=-1, keepdims=True)
    o = jnp.einsum('bhrnqk,bhrnkd->bhrnqd', (e / den).astype(v.dtype), vb)
    lse = (m + jnp.log(den))[..., 0]
    o = o.reshape(b, h, dilation, l, dh).transpose(0, 1, 3, 2, 4).reshape(b, h, s_pad, dh)[:, :, :s]
    lse = lse.reshape(b, h, dilation, l).transpose(0, 1, 3, 2).reshape(b, h, s_pad)[:, :, :s]
    return o, lse


def hierarchical_moe(h, w_rg, b_rg, w_re, b_re, w_gate, w_up, w_down):
    b, s, d = h.shape
    hf = h.reshape(b * s, d)
    h32 = hf.astype(jnp.float32)
    g_logits = h32 @ w_rg.astype(jnp.float32) + b_rg.astype(jnp.float32)
    g_probs = jax.nn.softmax(g_logits, axis=-1)
    g_sel = jnp.argmax(g_logits, axis=-1)
    g_w = jnp.take_along_axis(g_probs, g_sel[:, None], axis=-1)
    e_logits = jnp.einsum('nd,gde->nge', h32, w_re.astype(jnp.float32)) + b_re.astype(jnp.float32)
    e_logits = jnp.take_along_axis(e_logits, g_sel[:, None, None], axis=1)[:, 0]
    top_v, top_i = lax.top_k(e_logits, TOP_K_IN_GROUP)
    top_w = jax.nn.softmax(top_v, axis=-1) * g_w
    expert_id = g_sel[:, None] * EXPERTS_PER_GROUP + top_i
    gates = jnp.sum(jax.nn.one_hot(expert_id, N_EXPERTS, dtype=jnp.float32) * top_w[..., None], axis=1)
    gates = gates.astype(h.dtype)
    y = jnp.zeros_like(hf)
    for e in range(N_EXPERTS):
        a = jax.nn.silu(hf @ w_gate[e]) * (hf @ w_up[e])
        y = y + gates[:, e:e + 1] * (a @ w_down[e])
    return y.reshape(b, s, d)


def hybrid_layer(x, p_l, g_attn, w_qkv, q_norm_na, k_norm_na, rpb_na, q_norm_dil, k_norm_dil,
                 g_out_na, g_out_dil, w_o, g_ffn, w_router_group, b_router_group,
                 w_router_expert, b_router_expert, w_exp_gate, w_exp_up, w_exp_down,
                 g_ple, w_ple_gate, w_ple_proj):
    b, s, d = x.shape
    scale = HEAD_DIM ** -0.5
    h = rms_norm(x, g_attn)
    qkv = jnp.einsum('bsd,de->bse', h, w_qkv).reshape(b, s, 3, N_HEADS, HEAD_DIM)
    q = qkv[:, :, 0].transpose(0, 2, 1, 3)
    k = qkv[:, :, 1].transpose(0, 2, 1, 3)
    v = qkv[:, :, 2].transpose(0, 2, 1, 3)
    qa = rms_norm(q[:, :N_HEADS_NA], q_norm_na) * scale
    ka = rms_norm(k[:, :N_HEADS_NA], k_norm_na)
    out_a = neighbourhood_attention(qa, ka, v[:, :N_HEADS_NA], rpb_na)
    pos = jnp.arange(s)
    qb = rope(rms_norm(q[:, N_HEADS_NA:], q_norm_dil), pos) * scale
    kb = rope(rms_norm(k[:, N_HEADS_NA:], k_norm_dil), pos)
    vb = v[:, N_HEADS_NA:]
    outs = []
    lses = []
    for window, dilation in DIL_PAIRS:
        o_i, lse_i = dilated_window_attention(qb, kb, vb, window, dilation)
        outs.append(o_i)
        lses.append(lse_i)
    wts = jax.nn.softmax(jnp.stack(lses, axis=0), axis=0).astype(vb.dtype)
    out_b = jnp.einsum('pbhs,pbhsd->bhsd', wts, jnp.stack(outs, axis=0))
    ya = rms_norm(out_a.transpose(0, 2, 1, 3).reshape(b, s, N_HEADS_NA * HEAD_DIM), g_out_na)
    yb = rms_norm(out_b.transpose(0, 2, 1, 3).reshape(b, s, N_HEADS_DIL * HEAD_DIM), g_out_dil)
    x = x + jnp.einsum('bse,ed->bsd', jnp.concatenate([ya, yb], axis=-1), w_o)
    x = x + hierarchical_moe(rms_norm(x, g_ffn), w_router_group, b_router_group, w_router_expert,
                             b_router_expert, w_exp_gate, w_exp_up, w_exp_down)
    gate = jax.nn.sigmoid(jnp.einsum('bsd,de->bse', rms_norm(x, g_ple), w_ple_gate))
    return x + gate * jnp.einsum('bsp,pd->bsd', p_l, w_ple_proj)


def setup_inputs(seed: int = 0) -> dict:
    key = jax.random.key(seed)
    ks = jax.random.split(key, 23)
    f32 = jnp.float32
    D = D_MODEL
    na_w = N_HEADS_NA * HEAD_DIM
    dil_w = N_HEADS_DIL * HEAD_DIM

    def nrm(k, shape, sc):
        return sc * jax.random.normal(k, shape, f32)

    def gain(k, shape):
        return 1.0 + 0.05 * jax.random.normal(k, shape, f32)

    return {
        "x": nrm(ks[0], (BATCH, SEQ, D), 1.0),
        "p": nrm(ks[1], (DEPTH, BATCH, SEQ, PLE_DIM), 1.0),
        "g_attn": gain(ks[2], (DEPTH, D)),
        "w_qkv": nrm(ks[3], (DEPTH, D, 3 * D), D ** -0.5),
        "q_norm_na": gain(ks[4], (DEPTH, HEAD_DIM)),
        "k_norm_na": gain(ks[5], (DEPTH, HEAD_DIM)),
        "rpb_na": nrm(ks[6], (DEPTH, N_HEADS_NA, 2 * NA_ROWS_MAX - 1, 2 * NA_COLS - 1), 0.5),
        "q_norm_dil": gain(ks[7], (DEPTH, HEAD_DIM)),
        "k_norm_dil": gain(ks[8], (DEPTH, HEAD_DIM)),
        "g_out_na": gain(ks[9], (DEPTH, na_w)),
        "g_out_dil": gain(ks[10], (DEPTH, dil_w)),
        "w_o": nrm(ks[11], (DEPTH, D, D), D ** -0.5),
        "g_ffn": gain(ks[12], (DEPTH, D)),
        "w_router_group": nrm(ks[13], (DEPTH, D, N_GROUPS), D ** -0.5),
        "b_router_group": nrm(ks[14], (DEPTH, N_GROUPS), 0.01),
        "w_router_expert": nrm(ks[15], (DEPTH, N_GROUPS, D, EXPERTS_PER_GROUP), D ** -0.5),
        "b_router_expert": nrm(ks[16], (DEPTH, N_GROUPS, EXPERTS_PER_GROUP), 0.01),
        "w_exp_gate": nrm(ks[17], (DEPTH, N_EXPERTS, D, D_EXPERT), D ** -0.5),
        "w_exp_up": nrm(ks[18], (DEPTH, N_EXPERTS, D, D_EXPERT), D ** -0.5),
        "w_exp_down": nrm(ks[19], (DEPTH, N_EXPERTS, D_EXPERT, D), D_EXPERT ** -0.5),
        "g_ple": gain(ks[20], (DEPTH, D)),
        "w_ple_gate": nrm(ks[21], (DEPTH, D, D), D ** -0.5),
        "w_ple_proj": nrm(ks[22], (DEPTH, PLE_DIM, D), PLE_DIM ** -0.5),
    }


def reference(x, p, g_attn, w_qkv, q_norm_na, k_norm_na, rpb_na, q_norm_dil, k_norm_dil,
              g_out_na, g_out_dil, w_o, g_ffn, w_router_group, b_router_group,
              w_router_expert, b_router_expert, w_exp_gate, w_exp_up, w_exp_down,
              g_ple, w_ple_gate, w_ple_proj):
    for i in range(DEPTH):
        x = hybrid_layer(x, p[i], g_attn[i], w_qkv[i], q_norm_na[i], k_norm_na[i], rpb_na[i],
                         q_norm_dil[i], k_norm_dil[i], g_out_na[i], g_out_dil[i], w_o[i],
                         g_ffn[i], w_router_group[i], b_router_group[i], w_router_expert[i],
                         b_router_expert[i], w_exp_gate[i], w_exp_up[i], w_exp_down[i],
                         g_ple[i], w_ple_gate[i], w_ple_proj[i])
    return x
```

```python
import contextlib
import numpy as np
import concourse.bass as bass
import concourse.mybir as mybir
from concourse.bass_utils import run_bass_kernel_spmd

F32 = mybir.dt.float32
BF16 = mybir.dt.bfloat16
U8 = mybir.dt.uint8
ALU = mybir.AluOpType
AF = mybir.ActivationFunctionType
AX = mybir.AxisListType

S = 2048
D = 1024
NCH = 16
NCORES = 8
EPS = 1e-6
SEM_EPOCH = 12000


class Buf:
    __slots__ = ("name", "last_w", "readers", "excl")

    def __init__(self, name="", excl=False):
        self.name = name
        self.last_w = None
        self.readers = []
        self.excl = excl


class Inst:
    __slots__ = ("eng", "fn", "deps", "is_dma", "dsem", "need_inc", "sem", "val")

    def __init__(self, eng, fn, is_dma, dsem):
        self.eng = eng
        self.fn = fn
        self.deps = []
        self.is_dma = is_dma
        self.dsem = dsem
        self.need_inc = False
        self.sem = None
        self.val = None


class DmaSem:
    def __init__(self, name, group=False):
        self.name = name
        self.group = group
        self.count = 0
        self.handle = None


class Prog:
    ENGS = ("pe", "act", "dve", "pool", "sp")

    def __init__(self, nc):
        self.nc = nc
        self.E = {"pe": nc.tensor, "act": nc.scalar, "dve": nc.vector, "pool": nc.gpsimd, "sp": nc.sync}
        self.insts = []
        self.dsems = []
        self.last = {}
        self.dmas_since = []
        self.bar_deps = []
        self.bar_pending = set()

    def dma_sem(self, name, group=False):
        s = DmaSem(name, group)
        self.dsems.append(s)
        return s

    def barrier(self):
        carry = list(self.bar_deps) if self.bar_pending else []
        self.bar_deps = list(self.last.values()) + list(self.dmas_since) + carry
        self.bar_pending = set(self.ENGS)
        self.dmas_since = []

    def op(self, eng, fn, reads=(), writes=(), dsem=None, indep=False):
        ins = Inst(eng, fn, dsem is not None, dsem)
        deps = {}
        xr = [b for b in reads if b.excl]
        if xr:
            writes = list(writes) + xr
        if indep:
            reads_d, writes_d = (), ()
        else:
            reads_d, writes_d = reads, writes
        for b in reads_d:
            if b.last_w is not None:
                deps[id(b.last_w)] = b.last_w
        for b in writes_d:
            if b.last_w is not None:
                deps[id(b.last_w)] = b.last_w
            for r in b.readers:
                deps[id(r)] = r
        if eng in self.bar_pending:
            self.bar_pending.discard(eng)
            for d in self.bar_deps:
                if d.is_dma or d.eng != eng:
                    deps[id(d)] = d
        for d in deps.values():
            if d is ins:
                continue
            if d.eng == "pe" and eng == "pe" and not d.is_dma and dsem is None:
                continue
            ins.deps.append(d)
            d.need_inc = True
        for b in reads:
            b.readers.append(ins)
        for b in writes:
            b.last_w = ins
            b.readers = []
        self.insts.append(ins)
        if ins.is_dma:
            self.dmas_since.append(ins)
        else:
            self.last[eng] = ins
        return ins

    def emit(self, stack, final_wait=()):
        nc = self.nc
        ecount = {k: 0 for k in self.E}
        eepoch = {k: 0 for k in self.E}
        for ins in self.insts:
            if ins.is_dma:
                ins.dsem.count += 16
                ins.sem = ins.dsem
                ins.val = ins.dsem.count
            elif ins.need_inc:
                if ecount[ins.eng] >= SEM_EPOCH:
                    ecount[ins.eng] = 0
                    eepoch[ins.eng] += 1
                ecount[ins.eng] += 1
                ins.sem = (ins.eng, eepoch[ins.eng])
                ins.val = ecount[ins.eng]
        handles = {}
        for k in self.E:
            for ep in range(eepoch[k] + 1):
                handles[(k, ep)] = stack.enter_context(nc.semaphore(f"s_{k}_{ep}"))
        for s in self.dsems:
            s.handle = stack.enter_context(nc.semaphore(f"d_{s.name}"))
        self.nsem = len(handles) + len(self.dsems)
        waited = {k: {} for k in self.E}
        nwait = 0
        for ins in self.insts:
            e = self.E[ins.eng]
            need = {}
            for d in ins.deps:
                key = d.sem
                val = d.sem.count if (d.is_dma and d.sem.group) else d.val
                if need.get(key, 0) < val:
                    need[key] = val
            w = waited[ins.eng]
            for key, val in need.items():
                if w.get(key, 0) >= val:
                    continue
                w[key] = val
                h = key.handle if isinstance(key, DmaSem) else handles[key]
                e.wait_ge(h, val)
                nwait += 1
            bi = ins.fn(e)
            if ins.is_dma:
                bi.then_inc(ins.dsem.handle, 16)
            elif ins.need_inc:
                bi.then_inc(handles[ins.sem], 1)
        for s in final_wait:
            self.E["sp"].wait_ge(s.handle, s.count)
        self.nwait = nwait


class Arena:
    def __init__(self, nc, stack, nbytes):
        self.t = stack.enter_context(nc.sbuf_tensor("arena", [128, nbytes], U8))
        self.n = nbytes
        self.top = 0
        self.peak = 0

    def alloc(self, shape, dtype):
        isz = 4 if dtype == F32 else 2
        n = int(np.prod(shape))
        size = (n * isz + 63) // 64 * 64
        off = self.top
        self.top += size
        self.peak = max(self.peak, self.top)
        assert self.top <= self.n, f"SBUF arena overflow {self.top} > {self.n}"
        ap = self.t[:, off:off + n * isz].bitcast(dtype)
        if len(shape) == 2:
            ap = ap.rearrange("p (a b) -> p a b", a=shape[0])
        elif len(shape) == 3:
            ap = ap.rearrange("p (a b c) -> p a b c", a=shape[0], b=shape[1])
        return ap


def _rope_tables():
    half = 32
    inv = np.float32(10000.0) ** (-(np.arange(half, dtype=np.float32) / np.float32(half)))
    ang = np.arange(S, dtype=np.float32)[:, None] * inv[None, :].astype(np.float32)
    return np.cos(ang).astype(np.float32), np.sin(ang).astype(np.float32)


def _dil_mask():
    p = np.arange(128)[:, None]
    f = np.arange(128)[None, :]
    m = np.zeros((128, 23, 128), np.float32)
    for i in range(23):
        off = 128 * (i - 11) + p - f
        a = np.abs(off)
        m[:, i, :] = (a <= 64).astype(np.float32) + ((off % 4 == 0) & (a <= 256)) + ((off % 16 == 0) & (a <= 1024))
    return m


def _na_structure():
    rows, W = 32, 64
    row_start = np.clip(np.arange(rows) - 4, 0, rows - 8)
    col_start = np.clip(np.arange(W) - 8, 0, W - 16)
    kc_ = np.arange(64)[:, None]
    c_ = np.arange(64)[None, :]
    colm = (kc_ >= col_start[c_]) & (kc_ < col_start[c_] + 16)
    tiles = []
    tdelta = []
    for dl in range(-3, 4):
        tiles.append(np.tile(colm, (2, 2)).astype(np.float32))
        tdelta.append(dl + 3)
    lists = []
    for qc in range(16):
        lst = []
        for kc in range(16):
            dl = kc - qc
            vm = np.zeros((128, 128), bool)
            for kr in range(2):
                for qr in range(2):
                    r2 = 2 * kc + kr
                    r = 2 * qc + qr
                    if row_start[r] <= r2 < row_start[r] + 8:
                        vm[kr * 64:(kr + 1) * 64, qr * 64:(qr + 1) * 64] = colm
            if not vm.any():
                continue
            assert abs(dl) <= 3
            vmf = vm.astype(np.float32)
            tid = None
            for t in range(len(tiles)):
                if tdelta[t] == dl + 3 and np.array_equal(tiles[t], vmf):
                    tid = t
                    break
            if tid is None:
                tiles.append(vmf)
                tdelta.append(dl + 3)
                tid = len(tiles) - 1
            lst.append((kc, tid))
        lists.append(lst)
    return np.stack(tiles, axis=1), tdelta, lists


def _na_bias_gather_index():
    p = np.arange(128)
    f = np.arange(128)
    kr = (p // 64)[:, None, None]
    kcol = (p % 64)[:, None, None]
    qr = (f // 64)[None, None, :]
    c = (f % 64)[None, None, :]
    dl = (np.arange(7) - 3)[None, :, None]
    ri = np.clip(2 * dl + kr - qr + 7, 0, 14) + 0 * c
    ci = np.clip(kcol - c + 15, 0, 30) + 0 * dl
    return ri.astype(np.int64), ci.astype(np.int64)


_NA_TILES, _NA_TDELTA, _NA_LISTS = _na_structure()
NT_NA = _NA_TILES.shape[1]


def _runs(ids):
    out = []
    s = 0
    for i in range(1, len(ids) + 1):
        if i == len(ids) or ids[i] != ids[i - 1] + 1:
            out.append((s, i - s))
            s = i
    return out


def build_program(nseq=4, dbg=False, n_experts=32, stop_after=None):
    nc = bass.Bass("TRN2", target_bir_lowering=False)

    def din(name, shape):
        return nc.dram_tensor(name, list(shape), F32, kind="ExternalInput").ap()

    x = din("x", [nseq, S, D])
    pin = din("p", [nseq, S, 256])
    g_attn = din("g_attn", [1, D])
    w_qkv = din("w_qkv", [D, 3 * D])
    c_vec = din("c_vec", [1, 2112])
    c_wr = din("c_wr", [128, 8 * 36])
    biasG = din("biasG", [8, 128, 7 * 128])
    w_o = din("w_o", [D, D])
    g_ffn = din("g_ffn", [1, D])
    w_eg = din("w_eg", [32, D, 512])
    w_eu = din("w_eu", [32, D, 512])
    w_ed = din("w_ed", [32, 512, D])
    g_ple = din("g_ple", [1, D])
    w_pg = din("w_pg", [D, D])
    w_pp = din("w_pp", [256, D])
    c_ident = din("c_ident", [128, 128])
    c_cos = din("c_cos", [S, 32])
    c_sin = din("c_sin", [S, 32])
    c_dil = din("c_dil", [128, 23 * 128])
    c_na = din("c_na", [128, NT_NA * 128])
    out = nc.dram_tensor("out", [nseq, S, D], F32, kind="ExternalOutput").ap()
    if dbg:
        dbg1 = nc.dram_tensor("dbg1", [nseq, S, D], F32, kind="ExternalOutput").ap()
        dbg2 = nc.dram_tensor("dbg2", [nseq, S, D], F32, kind="ExternalOutput").ap()

    P = Prog(nc)
    stack = contextlib.ExitStack()
    ar = Arena(nc, stack, 207 * 1024)
    ps = [stack.enter_context(nc.psum_tensor(f"ps{i}", [128, 512], F32))[:, :] for i in range(8)]
    bPS = [Buf(f"ps{i}", excl=True) for i in range(8)]

    def op(eng, fn, r=(), w=(), dsem=None, indep=False):
        return P.op(eng, fn, r, w, dsem, indep)

    def mm(o, lhsT, rhs, st, sp, r, w):
        op("pe", lambda e: e.matmul(o, lhsT=lhsT, rhs=rhs, start=st, stop=sp), r, w)

    def tr(o, i, idn, r, w):
        op("pe", lambda e: e.transpose(o, i, idn), r, w)

    def act(o, i, func, r, w, **kw):
        op("act", lambda e: e.activation(out=o, in_=i, func=func, **kw), r, w)

    def tt(eng, o, a, b, opx, r, w):
        op(eng, lambda e: e.tensor_tensor(out=o, in0=a, in1=b, op=opx), r, w)

    def ts(eng, o, a, s1, s2, op0, op1, r, w):
        if s2 is None:
            op(eng, lambda e: e.tensor_scalar(out=o, in0=a, scalar1=s1, scalar2=None, op0=op0), r, w)
        else:
            op(eng, lambda e: e.tensor_scalar(out=o, in0=a, scalar1=s1, scalar2=s2, op0=op0, op1=op1), r, w)

    def stt(o, a, sc, b, op0, op1, r, w):
        op("dve", lambda e: e.scalar_tensor_tensor(out=o, in0=a, scalar=sc, in1=b, op0=op0, op1=op1), r, w)

    def cp(eng, o, i, r, w):
        if eng == "act":
            op("act", lambda e: e.copy(out=o, in_=i), r, w)
        else:
            op(eng, lambda e: e.tensor_copy(out=o, in_=i), r, w)

    def red(o, i, opx, r, w):
        op("dve", lambda e: e.tensor_reduce(out=o, in_=i, axis=AX.X, op=opx), r, w)

    def recip(o, i, r, w):
        op("dve", lambda e: e.reciprocal(out=o, in_=i), r, w)

    def dma(eng, o, i, r, w, dsem, indep=False):
        op(eng, lambda e: e.dma_start(out=o, in_=i), r, w, dsem, indep)

    X = ar.alloc([NCH, D], F32)
    bX = [Buf(f"X{c}") for c in range(NCH)]
    ident_f = ar.alloc([128], F32)
    ident_b = ar.alloc([128], BF16)
    cos_t = ar.alloc([NCH, 32], F32)
    sin_t = ar.alloc([NCH, 32], F32)
    cvec = ar.alloc([2112], F32)
    gain_na = cvec[:, 0:512]
    gain_dil = cvec[:, 512:1024]
    gout = [cvec[:, 1024:1536], cvec[:, 1536:2048]]
    brow = cvec[:, 2048:2084]
    dilmask = ar.alloc([23, 128], BF16)
    namask = ar.alloc([NT_NA, 128], F32)
    Wr = ar.alloc([8, 36], F32)
    Lg = ar.alloc([NCH, 36], F32)
    gates = ar.alloc([NCH, 32], F32)
    ssq = ar.alloc([NCH], F32)
    rstd = ar.alloc([NCH], F32)
    bC = Buf("consts")
    bSsq, bRstd, bL, bGates, bRt = Buf("ssq"), Buf("rstd"), Buf("L"), Buf("gates"), Buf("rt")
    persist_top = ar.top

    sC = P.dma_sem("const", group=True)
    dma("sp", ident_f, c_ident[:, :], [], [bC], sC, True)
    dma("sp", cos_t, c_cos.rearrange("(c p) j -> p c j", p=128), [], [bC], sC, True)
    dma("sp", sin_t, c_sin.rearrange("(c p) j -> p c j", p=128), [], [bC], sC, True)
    dma("sp", namask, c_na.rearrange("p (t f) -> p t f", f=128), [], [bC], sC, True)
    sC2 = P.dma_sem("const2", group=True)
    bCp = Buf("constp")
    dma("pool", dilmask, c_dil.rearrange("p (t f) -> p t f", f=128), [], [bCp], sC2, True)
    dma("sp", cvec, c_vec[0:1, :].to_broadcast([128, 2112]), [], [bC], sC, True)
    dma("sp", Wr, c_wr.rearrange("p (k j) -> p k j", j=36), [], [bC], sC, True)
    bC2 = Buf("consts2")
    cp("dve", ident_b, ident_f, [bC], [bC2])
    ts("dve", gain_na[:, 0:256], gain_na[:, 0:256], 0.125, None, ALU.mult, None, [bC], [bC2])
    ts("dve", gain_dil[:, 0:256], gain_dil[:, 0:256], 0.125, None, ALU.mult, None, [bC], [bC2])
    CONST = [bC, bC2, bCp]

    sX = [P.dma_sem(f"x{q}") for q in range(4)]
    sG = P.dma_sem("gbc")
    sW = [P.dma_sem("wqkv0"), P.dma_sem("wqkv1")]
    sB = P.dma_sem("biasg")
    sE = [[P.dma_sem(f"wexp{i}_{m}") for m in range(3)] for i in range(2)]
    sPl = P.dma_sem("pl")
    sWp = P.dma_sem("wple")
    sO = [P.dma_sem("out0"), P.dma_sem("out1")]
    sD = P.dma_sem("dbg")

    def phase_begin():
        P.barrier()
        ar.top = persist_top

    def interleave(lists):
        n = max(len(l) for l in lists)
        for j in range(n):
            for l in lists:
                if j < len(l):
                    l[j]()

    def load_x(s, q):
        xv = x[s].rearrange("(c p) d -> p c d", p=128)
        dma("sp", X[:, 4 * q:4 * q + 4, :], xv[:, 4 * q:4 * q + 4, :], [],
            [bX[c] for c in range(4 * q, 4 * q + 4)], sX[q])

    def norm_phase(g_dram, hT, bHT, fp32_router):
        g_bc = ar.alloc([D], F32)
        bG = Buf("gbc")
        dma("sp", g_bc, g_dram[0:1, :].to_broadcast([128, D]), [], [bG], sG)
        bJ = Buf("junk")
        hdt = F32 if fp32_router else BF16
        NB = 3 if fp32_router else 4
        htmp = [ar.alloc([D], hdt) for _ in range(NB)]
        junk = htmp[0].bitcast(BF16)[:, 0:D] if fp32_router else htmp[0]
        bH = [Buf() for _ in range(NB)]
        if fp32_router:
            hTf = [ar.alloc([8, 128], F32) for _ in range(NB)]
            bHf = [Buf() for _ in range(NB)]
        for c in range(NCH):
            act(junk, X[:, c, :], AF.Square, [bX[c]], [bJ, bSsq], accum_out=ssq[:, c:c + 1])
        act(rstd, ssq, AF.Sqrt, [bSsq], [bRstd], scale=1.0 / D, bias=EPS)
        recip(rstd, rstd, [bRstd], [bRstd])
        psT = [ps[4 + j][:, :].bitcast(BF16) for j in range(4)]

        def stage_a(c):
            i = c % NB
            stt(htmp[i], X[:, c, :], rstd[:, c:c + 1], g_bc, ALU.mult, ALU.mult, [bX[c], bRstd, bG], [bH[i]])

        def stage_b(c):
            i = c % NB
            hb = htmp[i]
            if not fp32_router:
                pt = psT[i]
                bp = bPS[4 + i]
                for k in range(8):
                    tr(pt[:, k * 128:(k + 1) * 128], hb[:, k * 128:(k + 1) * 128], ident_b, [bH[i]] + CONST, [bp])
                cp("act", hT[:, :, c * 128:(c + 1) * 128], pt.rearrange("p (k t) -> p k t", k=8), [bp], [bHT[c]])
            else:
                b0 = 2 * i
                for k in range(8):
                    pb = ps[b0 + k // 4]
                    tr(pb[:, (k % 4) * 128:(k % 4 + 1) * 128], hb[:, k * 128:(k + 1) * 128], ident_f,
                       [bH[i]] + CONST, [bPS[b0 + k // 4]])
                hf = hTf[i]
                for half in range(2):
                    pb = ps[b0 + half]
                    cp("dve", hf[:, 4 * half:4 * half + 4, :], pb.rearrange("p (k t) -> p k t", k=4),
                       [bPS[b0 + half]], [bHf[i]])
                    cp("act", hT[:, 4 * half:4 * half + 4, c * 128:(c + 1) * 128],
                       hf[:, 4 * half:4 * half + 4, :], [bHf[i]], [bHT[c]])
                for k in range(8):
                    mm(ps[6][:, 0:36], hf[:, k, :], Wr[:, k, :], k == 0, k == 7, [bHf[i]] + CONST, [bPS[6]])
                cp("dve", Lg[:, c, :], ps[6][:, 0:36], [bPS[6]], [bL])

        LA = NB - 1
        for c in range(NCH + LA):
            if c < NCH:
                stage_a(c)
            if c >= LA:
                stage_b(c - LA)

    def router_math():
        rtmp = ar.alloc([10, NCH, 8], F32)
        R = [rtmp[:, i, :, :] for i in range(10)]
        rd = [bL, bRt] + CONST
        wr = [bRt]
        L = Lg

        def b3(ap2, n):
            return ap2.unsqueeze(2).to_broadcast([128, NCH, n])

        tt("dve", L, L, brow.unsqueeze(1).to_broadcast([128, NCH, 36]), ALU.add, rd, [bL])
        gl = L[:, :, 0:4]
        gmax = R[0][:, :, 0]
        red(gmax, gl, ALU.max, rd, wr)
        goh = R[1][:, :, 0:4]
        tt("dve", goh, gl, b3(gmax, 4), ALU.is_equal, rd, wr)
        gsh = R[2][:, :, 0:4]
        tt("dve", gsh, gl, b3(gmax, 4), ALU.subtract, rd, wr)
        act(gsh, gsh, AF.Exp, rd, wr)
        gsum = R[0][:, :, 1]
        red(gsum, gsh, ALU.add, rd, wr)
        gw = R[0][:, :, 2]
        recip(gw, gsum, rd, wr)
        el = R[3]
        tmp = R[4]
        for g in range(4):
            src = L[:, :, 4 + 8 * g:12 + 8 * g]
            if g == 0:
                tt("dve", el, src, b3(goh[:, :, 0], 8), ALU.mult, rd, wr)
            else:
                tt("dve", tmp, src, b3(goh[:, :, g], 8), ALU.mult, rd, wr)
                tt("dve", el, el, tmp, ALU.add, rd, wr)
        m1 = R[0][:, :, 3]
        red(m1, el, ALU.max, rd, wr)
        oh1 = R[5]
        tt("dve", oh1, el, b3(m1, 8), ALU.is_equal, rd, wr)
        el2 = R[6]
        ts("dve", tmp, oh1, -1e30, None, ALU.mult, None, rd, wr)
        tt("dve", el2, tmp, el, ALU.add, rd, wr)
        m2 = R[0][:, :, 4]
        red(m2, el2, ALU.max, rd, wr)
        oh2 = R[7]
        tt("dve", oh2, el2, b3(m2, 8), ALU.is_equal, rd, wr)
        dm = R[0][:, :, 5]
        tt("dve", dm, m2, m1, ALU.subtract, rd, wr)
        e21 = R[0][:, :, 6]
        act(e21, dm, AF.Exp, rd, wr)
        den = R[0][:, :, 7]
        ts("dve", den, e21, 1.0, None, ALU.add, None, rd, wr)
        w1 = R[8][:, :, 0]
        recip(w1, den, rd, wr)
        tt("dve", w1, w1, gw, ALU.mult, rd, wr)
        w2 = R[8][:, :, 1]
        tt("dve", w2, e21, w1, ALU.mult, rd, wr)
        g8 = R[9]
        tt("dve", g8, oh1, b3(w1, 8), ALU.mult, rd, wr)
        tt("dve", tmp, oh2, b3(w2, 8), ALU.mult, rd, wr)
        tt("dve", g8, g8, tmp, ALU.add, rd, wr)
        for g in range(4):
            tt("dve", gates[:, :, 8 * g:8 * g + 8], g8, b3(goh[:, :, g], 8), ALU.mult, rd, [bGates])

    def wt_views(raw):
        return raw.rearrange("p (k n) -> p k n", k=8), raw[:, 0:4096].rearrange("p (k n) -> p k n", k=4)

    def load_qkv_w(qp, raw, bW, sem):
        group = qp // 2
        hbase = 8 * group + 4 * (qp % 2)
        Wt = wt_views(raw)[0]
        wv = w_qkv.rearrange("(k p) n -> p k n", p=128)
        for part in range(3):
            col = part * D + hbase * 64
            dma("pool", Wt[:, :, part * 256:(part + 1) * 256], wv[:, :, col:col + 256], [], [bW], sem)

    def load_wo(group, raw, bW, sem):
        Wo = wt_views(raw)[1]
        dma("pool", Wo, w_o[group * 512:(group + 1) * 512, :].rearrange("(k p) n -> p k n", p=128), [], [bW], sem)

    def attn_pass(qp, hT, bHT, outg, bOut, Wraw, bWt):
        group = qp // 2
        hbase = 8 * group + 4 * (qp % 2)
        is_na = group == 0
        QKT = ar.alloc([4, S], BF16)
        QT = QKT[:, 0:2, :]
        KT = QKT[:, 2:4, :]
        Vx = ar.alloc([NCH, 4, 65], BF16)
        mark_t = ar.top
        Wt = wt_views(Wraw[qp % 2])[0]
        bW = bWt[qp % 2]
        bQT = [Buf(f"qt{c}") for c in range(NCH)]
        bKT = [Buf(f"kt{c}") for c in range(NCH)]
        bV = [Buf(f"v{c}") for c in range(NCH)]
        op("pool", lambda e: e.memset(Vx[:, :, :, 64:65], 1.0), [], bV)
        sq = [ar.alloc([512], F32), ar.alloc([512], F32)]
        qn = [ar.alloc([512], F32), ar.alloc([512], F32)]
        qb = [ar.alloc([512], BF16), ar.alloc([512], BF16)]
        s8 = [ar.alloc([8], F32), ar.alloc([8], F32)]
        bSq, bQn, bQb, bS8 = ([Buf(), Buf()] for _ in range(4))
        if not is_na:
            rt_ = [ar.alloc([4, 8, 32], F32), ar.alloc([4, 8, 32], F32)]
            bRp = [Buf(), Buf()]
        gain = gain_na if is_na else gain_dil
        psT = [ps[6][:, :].bitcast(BF16), ps[7][:, :].bitcast(BF16)]

        def qkv_mm(c):
            pqk, pv = ps[c % 2], ps[2 + c % 2]
            for part in range(3):
                o = pqk[:, part * 256:(part + 1) * 256] if part < 2 else pv[:, 0:256]
                bo = bPS[c % 2] if part < 2 else bPS[2 + c % 2]
                for k in range(8):
                    mm(o, hT[:, k, c * 128:(c + 1) * 128], Wt[:, k, part * 256:(part + 1) * 256],
                       k == 0, k == 7, [bHT[c], bW], [bo])

        def post_steps(c):
            i = c % 2
            pqk, pv = ps[i], ps[2 + i]
            st = []
            st.append(lambda: act(sq[i], pqk, AF.Square, [bPS[i]], [bSq[i]]))
            st.append(lambda: cp("act", Vx[:, c, :, 0:64], pv[:, 0:256].rearrange("p (h d) -> p h d", h=4),
                                 [bPS[2 + i]], [bV[c]]))
            st.append(lambda: red(s8[i], sq[i].rearrange("p (h d) -> p h d", h=8), ALU.add, [bSq[i]], [bS8[i]]))
            st.append(lambda: act(s8[i], s8[i], AF.Sqrt, [bS8[i]], [bS8[i]], scale=1.0 / 64, bias=EPS))
            st.append(lambda: recip(s8[i], s8[i], [bS8[i]], [bS8[i]]))
            st.append(lambda: tt("dve", qn[i].rearrange("p (h d) -> p h d", h=8),
                                 pqk.rearrange("p (h d) -> p h d", h=8),
                                 s8[i].unsqueeze(2).to_broadcast([128, 8, 64]), ALU.mult,
                                 [bPS[i], bS8[i]], [bQn[i]]))
            if is_na:
                st.append(lambda: tt("dve", qb[i], qn[i], gain, ALU.mult, [bQn[i]] + CONST, [bQb[i]]))
            else:
                st.append(lambda: tt("dve", sq[i], qn[i], gain, ALU.mult, [bQn[i]] + CONST, [bSq[i]]))
                v = sq[i].rearrange("p (h t j) -> p h t j", h=8, t=2)
                o = qb[i].rearrange("p (h t j) -> p h t j", h=8, t=2)
                x1, x2 = v[:, :, 0, :], v[:, :, 1, :]
                cb = cos_t[:, c, :].unsqueeze(1).to_broadcast([128, 8, 32])
                sb = sin_t[:, c, :].unsqueeze(1).to_broadcast([128, 8, 32])
                T = rt_[i]
                st.append(lambda: tt("dve", T[:, 0], x1, cb, ALU.mult, [bSq[i]] + CONST, [bRp[i]]))
                st.append(lambda: tt("dve", T[:, 1], x2, sb, ALU.mult, [bSq[i]] + CONST, [bRp[i]]))
                st.append(lambda: tt("dve", T[:, 2], x1, sb, ALU.mult, [bSq[i]] + CONST, [bRp[i]]))
                st.append(lambda: tt("dve", T[:, 3], x2, cb, ALU.mult, [bSq[i]] + CONST, [bRp[i]]))
                st.append(lambda: tt("dve", o[:, :, 0, :], T[:, 0], T[:, 1], ALU.subtract, [bRp[i]], [bQb[i]]))
                st.append(lambda: tt("dve", o[:, :, 1, :], T[:, 2], T[:, 3], ALU.add, [bRp[i]], [bQb[i]]))
            return st

        def qkv_tr(c):
            i = c % 2
            pt = psT[i]
            for j in range(4):
                tr(pt[:, j * 128:(j + 1) * 128], qb[i][:, j * 128:(j + 1) * 128], ident_b, [bQb[i]] + CONST, [bPS[6 + i]])
            cp("act", QKT[:, :, c * 128:(c + 1) * 128], pt[:, 0:512].rearrange("p (a t) -> p a t", a=4),
               [bPS[6 + i]], [bQT[c], bKT[c]])

        npair = NCH // 2
        for k in range(npair + 1):
            if k < npair:
                qkv_mm(2 * k)
                qkv_mm(2 * k + 1)
            if k >= 1:
                qkv_tr(2 * k - 2)
                qkv_tr(2 * k - 1)
            if k < npair:
                interleave([post_steps(2 * k), post_steps(2 * k + 1)])

        P.barrier()
        ar.top = mark_t
        if qp < 3:
            load_qkv_w(qp + 1, Wraw[(qp + 1) % 2], bWt[(qp + 1) % 2], sW[(qp + 1) % 2])
        if qp == 1:
            load_wo(0, Wraw[1], bWt[1], sW[1])
        if qp == 3:
            load_wo(1, Wraw[0], bWt[0], sW[0])
        NE = 4
        Ebuf = [ar.alloc([512], BF16) for _ in range(NE)]
        Pbuf = [ar.alloc([512], BF16) for _ in range(NE)]
        bE = [Buf() for _ in range(NE)]
        bPm = [Buf() for _ in range(NE)]
        rden = [ar.alloc([1], F32), ar.alloc([1], F32)]
        bRd = [Buf(), Buf()]
        SB = [0, 1, 2, 3, 6, 7]
        if is_na:
            bias_t = ar.alloc([7, 128], F32)
            EB = [ar.alloc([NT_NA, 128], BF16), ar.alloc([NT_NA, 128], BF16)]
            bBias = Buf()
            bEB = [Buf(), Buf()]
        items = []
        hq = 0
        for hl in range(4):
            for qc in range(NCH):
                if is_na:
                    kl = _NA_LISTS[qc]
                else:
                    kl = [(kc, kc - qc + 11) for kc in range(max(0, qc - 8), min(15, qc + 8) + 1)]
                ngr = (len(kl) + 3) // 4
                for gi in range(ngr):
                    items.append((hl, qc, kl[4 * gi:4 * gi + 4], gi == 0, gi == ngr - 1, hq))
                hq += 1

        def prep_head(hl):
            h8 = hbase - 8 * group + hl
            dma("sp", bias_t, biasG[h8].rearrange("p (t f) -> p t f", f=128), [], [bBias], sB)
            act(bias_t, bias_t, AF.Exp, [bBias], [bBias])
            e = EB[hl % 2]
            tt("dve", e[:, 0:7, :], bias_t, namask[:, 0:7, :], ALU.mult, [bBias] + CONST, [bEB[hl % 2]])
            for t in range(7, NT_NA):
                tt("dve", e[:, t, :], bias_t[:, _NA_TDELTA[t], :], namask[:, t, :], ALU.mult,
                   [bBias] + CONST, [bEB[hl % 2]])

        def emit_qk(idx):
            hl, qc, grp, first, last, hq_ = items[idx]
            if is_na and first and qc == 0:
                prep_head(hl)
            pair, sub = hl // 2, hl % 2
            rows = slice(64 * sub, 64 * sub + 64)
            bank = SB[idx % len(SB)]
            ng = len(grp)
            for j, (kc, _) in enumerate(grp):
                mm(ps[bank][:, j * 128:(j + 1) * 128], KT[rows, pair, kc * 128:(kc + 1) * 128],
                   QT[rows, pair, qc * 128:(qc + 1) * 128], True, True, [bKT[kc], bQT[qc]], [bPS[bank]])
            E = Ebuf[idx % NE]
            act(E[:, 0:ng * 128], ps[bank][:, 0:ng * 128], AF.Exp, [bPS[bank]], [bE[idx % NE]])
            Pm = Pbuf[idx % NE]
            tids = [t for _, t in grp]
            for (s0, ln) in _runs(tids):
                t0 = tids[s0]
                if is_na:
                    m = EB[hl % 2][:, t0:t0 + ln, :]
                    rdm = [bEB[hl % 2]]
                else:
                    m = dilmask[:, t0:t0 + ln, :]
                    rdm = CONST
                tt("dve", Pm[:, s0 * 128:(s0 + ln) * 128].rearrange("p (t f) -> p t f", f=128),
                   E[:, s0 * 128:(s0 + ln) * 128].rearrange("p (t f) -> p t f", f=128), m, ALU.mult,
                   [bE[idx % NE]] + rdm, [bPm[idx % NE]])

        def emit_pv(idx):
            hl, qc, grp, first, last, hq_ = items[idx]
            ab = 4 + hq_ % 2
            ng = len(grp)
            Pm = Pbuf[idx % NE]
            for j, (kc, _) in enumerate(grp):
                mm(ps[ab][:, 0:65], Pm[:, j * 128:(j + 1) * 128], Vx[:, kc, hl, :],
                   first and j == 0, last and j == ng - 1, [bPm[idx % NE], bV[kc]], [bPS[ab]])
            if last:
                r = rden[hq_ % 2]
                recip(r, ps[ab][:, 64:65], [bPS[ab]], [bRd[hq_ % 2]])
                hcol = (hbase - 8 * group + hl) * 64
                ts("dve", outg[:, qc, hcol:hcol + 64], ps[ab][:, 0:64], r, None, ALU.mult, None,
                   [bPS[ab], bRd[hq_ % 2]], [bOut[qc]])

        LAG = 3
        for i in range(len(items) + LAG):
            if i < len(items):
                emit_qk(i)
            if i >= LAG:
                emit_pv(i - LAG)

    def wo_phase(group, outg, bOut, Wo, bWo):
        yT = ar.alloc([4, S], BF16)
        junk = ar.alloc([512], BF16)
        ytmp = [ar.alloc([512], BF16) for _ in range(4)]
        bYT = [Buf() for _ in range(NCH)]
        bJ = Buf()
        bY = [Buf() for _ in range(4)]
        for c in range(NCH):
            act(junk, outg[:, c, :], AF.Square, [bOut[c]], [bJ, bSsq], accum_out=ssq[:, c:c + 1])
        act(rstd, ssq, AF.Sqrt, [bSsq], [bRstd], scale=1.0 / 512, bias=EPS)
        recip(rstd, rstd, [bRstd], [bRstd])
        psT = [ps[4 + j][:, :].bitcast(BF16) for j in range(4)]

        def sa(c):
            i = c % 4
            stt(ytmp[i], outg[:, c, :], rstd[:, c:c + 1], gout[group], ALU.mult, ALU.mult,
                [bOut[c], bRstd] + CONST, [bY[i]])

        def sb_(c):
            i = c % 4
            for k in range(4):
                tr(psT[i][:, k * 128:(k + 1) * 128], ytmp[i][:, k * 128:(k + 1) * 128], ident_b,
                   [bY[i]] + CONST, [bPS[4 + i]])
            cp("act", yT[:, :, c * 128:(c + 1) * 128], psT[i][:, 0:512].rearrange("p (k t) -> p k t", k=4),
               [bPS[4 + i]], [bYT[c]])

        def sc_(c):
            for db in range(2):
                b = (2 * c + db) % 4
                for k in range(4):
                    mm(ps[b][:, :], yT[:, k, c * 128:(c + 1) * 128], Wo[:, k, db * 512:(db + 1) * 512],
                       k == 0, k == 3, [bYT[c], bWo], [bPS[b]])
                xs = X[:, c, db * 512:(db + 1) * 512]
                tt("dve", xs, ps[b][:, :], xs, ALU.add, [bPS[b], bX[c]], [bX[c]])

        for c in range(NCH + 5):
            if c < NCH:
                sa(c)
            if 3 <= c < NCH + 3:
                sb_(c - 3)
            if c >= 5:
                sc_(c - 5)

    def moe_alloc():
        mb = dict(
            Wg=[ar.alloc([8, 512], BF16) for _ in range(2)],
            Wu=[ar.alloc([8, 512], BF16) for _ in range(2)],
            Wd=[ar.alloc([4, D], BF16) for _ in range(2)],
            bWg=[Buf(), Buf()], bWu=[Buf(), Buf()], bWd=[Buf(), Buf()],
        )
        return mb

    def moe_load(mb, e):
        s = e % 2
        dma("pool", mb["Wg"][s], w_eg[e].rearrange("(k p) f -> p k f", p=128), [], [mb["bWg"][s]], sE[s][0])
        dma("pool", mb["Wu"][s], w_eu[e].rearrange("(k p) f -> p k f", p=128), [], [mb["bWu"][s]], sE[s][1])
        dma("pool", mb["Wd"][s], w_ed[e].rearrange("(f p) d -> p f d", p=128), [], [mb["bWd"][s]], sE[s][2])

    def moe_phase(hT, bHT, mb):
        Wg, Wu, Wd = mb["Wg"], mb["Wu"], mb["Wd"]
        aT = ar.alloc([4, S], BF16)
        sl = [ar.alloc([512], BF16), ar.alloc([512], BF16)]
        bWg, bWu, bWd = mb["bWg"], mb["bWu"], mb["bWd"]
        bA = [[Buf() for _ in range(4)] for _ in range(4)]
        bSl = [Buf(), Buf()]
        cnt = {"gu": 0, "dn": 0}

        def gu(e, tb):
            s = e % 2
            for f in range(4):
                n = cnt["gu"]
                cnt["gu"] += 1
                pg, pu = ps[n % 2], ps[2 + n % 2]
                rd = [bHT[4 * tb + i] for i in range(4)]
                for k in range(8):
                    mm(pg[:, :], Wg[s][:, k, f * 128:(f + 1) * 128], hT[:, k, tb * 512:(tb + 1) * 512],
                       k == 0, k == 7, rd + [bWg[s]], [bPS[n % 2]])
                for k in range(8):
                    mm(pu[:, :], Wu[s][:, k, f * 128:(f + 1) * 128], hT[:, k, tb * 512:(tb + 1) * 512],
                       k == 0, k == 7, rd + [bWu[s]], [bPS[2 + n % 2]])
                act(sl[n % 2], pg[:, :], AF.Silu, [bPS[n % 2]], [bSl[n % 2]])
                tt("dve", aT[:, f, tb * 512:(tb + 1) * 512], pu[:, :], sl[n % 2], ALU.mult,
                   [bPS[2 + n % 2], bSl[n % 2]], [bA[f][tb]])

        def down(e, tb):
            s = e % 2
            for cc in range(4):
                c = 4 * tb + cc
                for db in range(2):
                    n = cnt["dn"]
                    cnt["dn"] += 1
                    b = 4 + n % 2
                    for f in range(4):
                        mm(ps[b][:, :], aT[:, f, c * 128:(c + 1) * 128], Wd[s][:, f, db * 512:(db + 1) * 512],
                           f == 0, f == 3, [bA[f][tb], bWd[s]], [bPS[b]])
                    xs = X[:, c, db * 512:(db + 1) * 512]
                    stt(xs, ps[b][:, :], gates[:, c, e:e + 1], xs, ALU.mult, ALU.add,
                        [bPS[b], bGates, bX[c]], [bX[c]])

        tasks = []
        for e in range(n_experts):
            for tb in range(4):
                tasks.append((e, tb))
        for i in range(len(tasks) + 1):
            if i < len(tasks):
                gu(*tasks[i])
            if i >= 1:
                e, tb = tasks[i - 1]
                down(e, tb)
                if tb == 3 and e + 2 < n_experts:
                    moe_load(mb, e + 2)

    def tail_alloc(s):
        tb = dict(pl=ar.alloc([NCH, 256], F32), pT=ar.alloc([2, S], BF16), Wpg=ar.alloc([8, D], BF16),
                  Wpp=ar.alloc([2, D], BF16), bPl=Buf(), bW=Buf())
        dma("sp", tb["pl"], pin[s].rearrange("(c p) j -> p c j", p=128), [], [tb["bPl"]], sPl)
        dma("pool", tb["Wpg"], w_pg.rearrange("(k p) n -> p k n", p=128), [], [tb["bW"]], sWp)
        dma("pool", tb["Wpp"], w_pp.rearrange("(k p) n -> p k n", p=128), [], [tb["bW"]], sWp)
        return tb

    def tail_phase(s, hT, bHT, tb):
        pl, pT, Wpg, Wpp, bPl, bW = tb["pl"], tb["pT"], tb["Wpg"], tb["Wpp"], tb["bPl"], tb["bW"]
        sg = [ar.alloc([512], F32), ar.alloc([512], F32)]
        ost = [ar.alloc([D], F32), ar.alloc([D], F32)]
        bPT = [Buf() for _ in range(NCH)]
        bSg = [Buf(), Buf()]
        bOs = [Buf(), Buf()]
        for c in range(NCH):
            b = 6 + c % 2
            for k in range(2):
                tr(ps[b][:, k * 128:(k + 1) * 128], pl[:, c, k * 128:(k + 1) * 128], ident_f, [bPl] + CONST, [bPS[b]])
            cp("act", pT[:, :, c * 128:(c + 1) * 128], ps[b][:, 0:256].rearrange("p (k t) -> p k t", k=2),
               [bPS[b]], [bPT[c]])
        n = 0
        for c in range(NCH):
            o = ost[c % 2]
            for db in range(2):
                i = n % 2
                n += 1
                pg, pp = ps[i], ps[2 + i]
                for k in range(8):
                    mm(pg[:, :], hT[:, k, c * 128:(c + 1) * 128], Wpg[:, k, db * 512:(db + 1) * 512],
                       k == 0, k == 7, [bHT[c], bW], [bPS[i]])
                for k in range(2):
                    mm(pp[:, :], pT[:, k, c * 128:(c + 1) * 128], Wpp[:, k, db * 512:(db + 1) * 512],
                       k == 0, k == 1, [bPT[c], bW], [bPS[2 + i]])
                act(sg[i], pg[:, :], AF.Sigmoid, [bPS[i]], [bSg[i]])
                tt("dve", sg[i], pp[:, :], sg[i], ALU.mult, [bPS[2 + i], bSg[i]], [bSg[i]])
                tt("dve", o[:, db * 512:(db + 1) * 512], sg[i], X[:, c, db * 512:(db + 1) * 512], ALU.add,
                   [bSg[i], bX[c]], [bOs[c % 2]])
            dma("sp", out[s, c * 128:(c + 1) * 128, :], o, [bOs[c % 2]], [Buf()], sO[c % 2])
            if c % 4 == 3 and s + 1 < nseq:
                load_x(s + 1, c // 4)

    def dump_x(dst, s):
        P.barrier()
        bd = Buf()
        for q in range(4):
            dma("sp", dst[s].rearrange("(c p) d -> p c d", p=128)[:, 4 * q:4 * q + 4, :], X[:, 4 * q:4 * q + 4, :],
                [bX[c] for c in range(4 * q, 4 * q + 4)], [bd], sD)
        P.barrier()

    class _Stop(Exception):
        pass

    nph = [0]

    def tick():
        nph[0] += 1
        if stop_after is not None and nph[0] >= stop_after:
            raise _Stop()

    try:
        for s in range(nseq):
            if s == 0:
                for q in range(4):
                    load_x(0, q)
            tick()
            phase_begin()
            hT = ar.alloc([8, S], BF16)
            bHT = [Buf(f"hT{c}") for c in range(NCH)]
            Wraw = [ar.alloc([6144], BF16), ar.alloc([6144], BF16)]
            bWt = [Buf("wt0"), Buf("wt1")]
            load_qkv_w(0, Wraw[0], bWt[0], sW[0])
            mark_h = ar.top
            norm_phase(g_attn, hT, bHT, False)
            tick()
            for group in range(2):
                P.barrier()
                ar.top = mark_h
                outg = ar.alloc([NCH, 512], BF16)
                bOut = [Buf(f"og{c}") for c in range(NCH)]
                mark_o = ar.top
                for sub in range(2):
                    P.barrier()
                    ar.top = mark_o
                    attn_pass(2 * group + sub, hT, bHT, outg, bOut, Wraw, bWt)
                    tick()
                P.barrier()
                ar.top = mark_o
                wslot = 1 if group == 0 else 0
                wo_phase(group, outg, bOut, wt_views(Wraw[wslot])[1], bWt[wslot])
                tick()
            if dbg:
                dump_x(dbg1, s)
            phase_begin()
            hT = ar.alloc([8, S], BF16)
            bHT = [Buf(f"h2T{c}") for c in range(NCH)]
            mb = moe_alloc()
            moe_load(mb, 0)
            if n_experts > 1:
                moe_load(mb, 1)
            mark_h = ar.top
            norm_phase(g_ffn, hT, bHT, True)
            tick()
            router_math()
            tick()
            P.barrier()
            ar.top = mark_h
            moe_phase(hT, bHT, mb)
            tick()
            if dbg:
                dump_x(dbg2, s)
            phase_begin()
            hT = ar.alloc([8, S], BF16)
            bHT = [Buf(f"h3T{c}") for c in range(NCH)]
            tbufs = tail_alloc(s)
            mark_h = ar.top
            norm_phase(g_ple, hT, bHT, False)
            tick()
            P.barrier()
            ar.top = mark_h
            tail_phase(s, hT, bHT, tbufs)
    except _Stop:
        P.barrier()
        for q in range(4):
            dma("sp", out[0].rearrange("(c p) d -> p c d", p=128)[:, 4 * q:4 * q + 4, :], X[:, 4 * q:4 * q + 4, :],
                [bX[c] for c in range(4 * q, 4 * q + 4)], [Buf()], sO[q % 2])

    fw = list(sO) + ([sD] if dbg else [])
    P.emit(stack, final_wait=fw)
    build_program.info = dict(ninst=len(P.insts), nwait=P.nwait, nsem=P.nsem, sbuf_peak=ar.peak)
    return nc


def _host_inputs(inp, nseq_per_core, ncores):
    f32 = lambda a: np.ascontiguousarray(np.asarray(a, dtype=np.float32))
    cos, sin = _rope_tables()
    ri, ci = _na_bias_gather_index()
    rpb = np.asarray(inp["rpb_na"], dtype=np.float32)[0]
    biasg = rpb[:, ri, ci].reshape(8, 128, 7 * 128)
    cvec = np.zeros((1, 2112), np.float32)
    cvec[0, 0:256] = np.tile(f32(inp["q_norm_na"]).reshape(64), 4)
    cvec[0, 256:512] = np.tile(f32(inp["k_norm_na"]).reshape(64), 4)
    cvec[0, 512:768] = np.tile(f32(inp["q_norm_dil"]).reshape(64), 4)
    cvec[0, 768:1024] = np.tile(f32(inp["k_norm_dil"]).reshape(64), 4)
    cvec[0, 1024:1536] = f32(inp["g_out_na"]).reshape(512)
    cvec[0, 1536:2048] = f32(inp["g_out_dil"]).reshape(512)
    cvec[0, 2048:2052] = f32(inp["b_router_group"]).reshape(4)
    cvec[0, 2052:2084] = f32(inp["b_router_expert"]).reshape(32)
    wcat = np.concatenate([f32(inp["w_router_group"][0])] + [f32(inp["w_router_expert"][0][g]) for g in range(4)],
                          axis=1)
    wr = np.ascontiguousarray(wcat.reshape(8, 128, 36).transpose(1, 0, 2).reshape(128, 8 * 36))
    shared = {
        "g_attn": f32(inp["g_attn"]).reshape(1, D),
        "w_qkv": f32(inp["w_qkv"][0]),
        "c_vec": cvec,
        "c_wr": wr,
        "biasG": f32(biasg),
        "w_o": f32(inp["w_o"][0]),
        "g_ffn": f32(inp["g_ffn"]).reshape(1, D),
        "w_eg": f32(inp["w_exp_gate"][0]),
        "w_eu": f32(inp["w_exp_up"][0]),
        "w_ed": f32(inp["w_exp_down"][0]),
        "g_ple": f32(inp["g_ple"]).reshape(1, D),
        "w_pg": f32(inp["w_ple_gate"][0]),
        "w_pp": f32(inp["w_ple_proj"][0]),
        "c_ident": np.eye(128, dtype=np.float32),
        "c_cos": cos,
        "c_sin": sin,
        "c_dil": _dil_mask().reshape(128, 23 * 128),
        "c_na": np.ascontiguousarray(_NA_TILES.reshape(128, NT_NA * 128)),
    }
    xs = f32(inp["x"])
    ps_ = f32(inp["p"][0])
    maps = []
    for i in range(ncores):
        m = dict(shared)
        m["x"] = xs[i * nseq_per_core:(i + 1) * nseq_per_core]
        m["p"] = ps_[i * nseq_per_core:(i + 1) * nseq_per_core]
        maps.append(m)
    return maps


def kernel(**inputs):
    B = np.asarray(inputs["x"]).shape[0]
    nseq = B // NCORES
    nc = build_program(nseq=nseq)
    maps = _host_inputs(inputs, nseq, NCORES)
    res = run_bass_kernel_spmd(nc, maps, core_ids=list(range(NCORES)))
    outs = [np.asarray(r["out"], dtype=np.float32) for r in res.results]
    return np.concatenate(outs, axis=0)
```

```python
import contextlib
import numpy as np
import concourse.bass as bass
import concourse.mybir as mybir
from concourse.bass_utils import run_bass_kernel_spmd

F32 = mybir.dt.float32
BF16 = mybir.dt.bfloat16
U8 = mybir.dt.uint8
ALU = mybir.AluOpType
AF = mybir.ActivationFunctionType
AX = mybir.AxisListType

S = 2048
D = 1024
NCH = 16
NCORES = 8
EPS = 1e-6
SEM_EPOCH = 12000


class Buf:
    __slots__ = ("name", "last_w", "readers", "excl")

    def __init__(self, name="", excl=False):
        self.name = name
        self.last_w = None
        self.readers = []
        self.excl = excl


class Inst:
    __slots__ = ("eng", "fn", "deps", "is_dma", "dsem", "need_inc", "sem", "val")

    def __init__(self, eng, fn, is_dma, dsem):
        self.eng = eng
        self.fn = fn
        self.deps = []
        self.is_dma = is_dma
        self.dsem = dsem
        self.need_inc = False
        self.sem = None
        self.val = None


class DmaSem:
    def __init__(self, name, group=False):
        self.name = name
        self.group = group
        self.count = 0
        self.handle = None


class Prog:
    ENGS = ("pe", "act", "dve", "pool", "sp")

    def __init__(self, nc):
        self.nc = nc
        self.E = {"pe": nc.tensor, "act": nc.scalar, "dve": nc.vector, "pool": nc.gpsimd, "sp": nc.sync}
        self.insts = []
        self.dsems = []
        self.last = {}
        self.dmas_since = []
        self.bar_deps = []
        self.bar_pending = set()

    def dma_sem(self, name, group=False):
        s = DmaSem(name, group)
        self.dsems.append(s)
        return s

    def barrier(self):
        carry = list(self.bar_deps) if self.bar_pending else []
        self.bar_deps = list(self.last.values()) + list(self.dmas_since) + carry
        self.bar_pending = set(self.ENGS)
        self.dmas_since = []

    def op(self, eng, fn, reads=(), writes=(), dsem=None, indep=False):
        ins = Inst(eng, fn, dsem is not None, dsem)
        deps = {}
        xr = [b for b in reads if b.excl]
        if xr:
            writes = list(writes) + xr
        if indep:
            reads_d, writes_d = (), ()
        else:
            reads_d, writes_d = reads, writes
        for b in reads_d:
            if b.last_w is not None:
                deps[id(b.last_w)] = b.last_w
        for b in writes_d:
            if b.last_w is not None:
                deps[id(b.last_w)] = b.last_w
            for r in b.readers:
                deps[id(r)] = r
        if eng in self.bar_pending:
            self.bar_pending.discard(eng)
            for d in self.bar_deps:
                if d.is_dma or d.eng != eng:
                    deps[id(d)] = d
        for d in deps.values():
            if d is ins:
                continue
            if d.eng == "pe" and eng == "pe" and not d.is_dma and dsem is None:
                continue
            ins.deps.append(d)
            d.need_inc = True
        for b in reads:
            b.readers.append(ins)
        for b in writes:
            b.last_w = ins
            b.readers = []
        self.insts.append(ins)
        if ins.is_dma:
            self.dmas_since.append(ins)
        else:
            self.last[eng] = ins
        return ins

    def emit(self, stack, final_wait=()):
        nc = self.nc
        ecount = {k: 0 for k in self.E}
        eepoch = {k: 0 for k in self.E}
        for ins in self.insts:
            if ins.is_dma:
                ins.dsem.count += 16
                ins.sem = ins.dsem
                ins.val = ins.dsem.count
            elif ins.need_inc:
                if ecount[ins.eng] >= SEM_EPOCH:
                    ecount[ins.eng] = 0
                    eepoch[ins.eng] += 1
                ecount[ins.eng] += 1
                ins.sem = (ins.eng, eepoch[ins.eng])
                ins.val = ecount[ins.eng]
        handles = {}
        for k in self.E:
            for ep in range(eepoch[k] + 1):
                handles[(k, ep)] = stack.enter_context(nc.semaphore(f"s_{k}_{ep}"))
        for s in self.dsems:
            s.handle = stack.enter_context(nc.semaphore(f"d_{s.name}"))
        self.nsem = len(handles) + len(self.dsems)
        waited = {k: {} for k in self.E}
        nwait = 0
        for ins in self.insts:
            e = self.E[ins.eng]
            need = {}
            for d in ins.deps:
                key = d.sem
                val = d.sem.count if (d.is_dma and d.sem.group) else d.val
                if need.get(key, 0) < val:
                    need[key] = val
            w = waited[ins.eng]
            for key, val in need.items():
                if w.get(key, 0) >= val:
                    continue
                w[key] = val
                h = key.handle if isinstance(key, DmaSem) else handles[key]
                e.wait_ge(h, val)
                nwait += 1
            bi = ins.fn(e)
            if ins.is_dma:
                bi.then_inc(ins.dsem.handle, 16)
            elif ins.need_inc:
                bi.then_inc(handles[ins.sem], 1)
        for s in final_wait:
            self.E["sp"].wait_ge(s.handle, s.count)
        self.nwait = nwait


class Arena:
    def __init__(self, nc, stack, nbytes):
        self.t = stack.enter_context(nc.sbuf_tensor("arena", [128, nbytes], U8))
        self.n = nbytes
        self.top = 0
        self.peak = 0

    def alloc(self, shape, dtype):
        isz = 4 if dtype == F32 else 2
        n = int(np.prod(shape))
        size = (n * isz + 63) // 64 * 64
        off = self.top
        self.top += size
        self.peak = max(self.peak, self.top)
        assert self.top <= self.n, f"SBUF arena overflow {self.top} > {self.n}"
        ap = self.t[:, off:off + n * isz].bitcast(dtype)
        if len(shape) == 2:
            ap = ap.rearrange("p (a b) -> p a b", a=shape[0])
        elif len(shape) == 3:
            ap = ap.rearrange("p (a b c) -> p a b c", a=shape[0], b=shape[1])
        return ap


def _rope_tables():
    half = 32
    inv = np.float32(10000.0) ** (-(np.arange(half, dtype=np.float32) / np.float32(half)))
    ang = np.arange(S, dtype=np.float32)[:, None] * inv[None, :].astype(np.float32)
    return np.cos(ang).astype(np.float32), np.sin(ang).astype(np.float32)


def _dil_mask():
    p = np.arange(128)[:, None]
    f = np.arange(128)[None, :]
    m = np.zeros((128, 23, 128), np.float32)
    for i in range(23):
        off = 128 * (i - 11) + p - f
        a = np.abs(off)
        m[:, i, :] = (a <= 64).astype(np.float32) + ((off % 4 == 0) & (a <= 256)) + ((off % 16 == 0) & (a <= 1024))
    return m


def _na_structure():
    rows, W = 32, 64
    row_start = np.clip(np.arange(rows) - 4, 0, rows - 8)
    col_start = np.clip(np.arange(W) - 8, 0, W - 16)
    kc_ = np.arange(64)[:, None]
    c_ = np.arange(64)[None, :]
    colm = (kc_ >= col_start[c_]) & (kc_ < col_start[c_] + 16)
    tiles = []
    tdelta = []
    for dl in range(-3, 4):
        tiles.append(np.tile(colm, (2, 2)).astype(np.float32))
        tdelta.append(dl + 3)
    lists = []
    for qc in range(16):
        lst = []
        for kc in range(16):
            dl = kc - qc
            vm = np.zeros((128, 128), bool)
            for kr in range(2):
                for qr in range(2):
                    r2 = 2 * kc + kr
                    r = 2 * qc + qr
                    if row_start[r] <= r2 < row_start[r] + 8:
                        vm[kr * 64:(kr + 1) * 64, qr * 64:(qr + 1) * 64] = colm
            if not vm.any():
                continue
            assert abs(dl) <= 3
            vmf = vm.astype(np.float32)
            tid = None
            for t in range(len(tiles)):
                if tdelta[t] == dl + 3 and np.array_equal(tiles[t], vmf):
                    tid = t
                    break
            if tid is None:
                tiles.append(vmf)
                tdelta.append(dl + 3)
                tid = len(tiles) - 1
            lst.append((kc, tid))
        lists.append(lst)
    return np.stack(tiles, axis=1), tdelta, lists


def _na_bias_gather_index():
    p = np.arange(128)
    f = np.arange(128)
    kr = (p // 64)[:, None, None]
    kcol = (p % 64)[:, None, None]
    qr = (f // 64)[None, None, :]
    c = (f % 64)[None, None, :]
    dl = (np.arange(7) - 3)[None, :, None]
    ri = np.clip(2 * dl + kr - qr + 7, 0, 14) + 0 * c
    ci = np.clip(kcol - c + 15, 0, 30) + 0 * dl
    return ri.astype(np.int64), ci.astype(np.int64)


_NA_TILES, _NA_TDELTA, _NA_LISTS = _na_structure()
NT_NA = _NA_TILES.shape[1]


def _runs(ids):
    out = []
    s = 0
    for i in range(1, len(ids) + 1):
        if i == len(ids) or ids[i] != ids[i - 1] + 1:
            out.append((s, i - s))
            s = i
    return out


def build_program(nseq=4, dbg=False, n_experts=32, stop_after=None):
    nc = bass.Bass("TRN2", target_bir_lowering=False)

    def din(name, shape):
        return nc.dram_tensor(name, list(shape), F32, kind="ExternalInput").ap()

    x = din("x", [nseq, S, D])
    pin = din("p", [nseq, S, 256])
    g_attn = din("g_attn", [1, D])
    w_qkv = din("w_qkv", [D, 3 * D])
    c_vec = din("c_vec", [1, 2112])
    c_wr = din("c_wr", [128, 8 * 36])
    biasG = din("biasG", [8, 128, 7 * 128])
    w_o = din("w_o", [D, D])
    g_ffn = din("g_ffn", [1, D])
    w_eg = din("w_eg", [32, D, 512])
    w_eu = din("w_eu", [32, D, 512])
    w_ed = din("w_ed", [32, 512, D])
    g_ple = din("g_ple", [1, D])
    w_pg = din("w_pg", [D, D])
    w_pp = din("w_pp", [256, D])
    c_ident = din("c_ident", [128, 128])
    c_cos = din("c_cos", [S, 32])
    c_sin = din("c_sin", [S, 32])
    c_dil = din("c_dil", [128, 23 * 128])
    c_na = din("c_na", [128, NT_NA * 128])
    out = nc.dram_tensor("out", [nseq, S, D], F32, kind="ExternalOutput").ap()
    if dbg:
        dbg1 = nc.dram_tensor("dbg1", [nseq, S, D], F32, kind="ExternalOutput").ap()
        dbg2 = nc.dram_tensor("dbg2", [nseq, S, D], F32, kind="ExternalOutput").ap()

    P = Prog(nc)
    stack = contextlib.ExitStack()
    ar = Arena(nc, stack, 207 * 1024)
    ps = [stack.enter_context(nc.psum_tensor(f"ps{i}", [128, 512], F32))[:, :] for i in range(8)]
    bPS = [Buf(f"ps{i}", excl=True) for i in range(8)]

    def op(eng, fn, r=(), w=(), dsem=None, indep=False):
        return P.op(eng, fn, r, w, dsem, indep)

    def mm(o, lhsT, rhs, st, sp, r, w):
        op("pe", lambda e: e.matmul(o, lhsT=lhsT, rhs=rhs, start=st, stop=sp), r, w)

    def tr(o, i, idn, r, w):
        op("pe", lambda e: e.transpose(o, i, idn), r, w)

    def act(o, i, func, r, w, **kw):
        op("act", lambda e: e.activation(out=o, in_=i, func=func, **kw), r, w)

    def tt(eng, o, a, b, opx, r, w):
        op(eng, lambda e: e.tensor_tensor(out=o, in0=a, in1=b, op=opx), r, w)

    def ts(eng, o, a, s1, s2, op0, op1, r, w):
        if s2 is None:
            op(eng, lambda e: e.tensor_scalar(out=o, in0=a, scalar1=s1, scalar2=None, op0=op0), r, w)
        else:
            op(eng, lambda e: e.tensor_scalar(out=o, in0=a, scalar1=s1, scalar2=s2, op0=op0, op1=op1), r, w)

    def stt(o, a, sc, b, op0, op1, r, w):
        op("dve", lambda e: e.scalar_tensor_tensor(out=o, in0=a, scalar=sc, in1=b, op0=op0, op1=op1), r, w)

    def cp(eng, o, i, r, w):
        if eng == "act":
            op("act", lambda e: e.copy(out=o, in_=i), r, w)
        else:
            op(eng, lambda e: e.tensor_copy(out=o, in_=i), r, w)

    def red(o, i, opx, r, w):
        op("dve", lambda e: e.tensor_reduce(out=o, in_=i, axis=AX.X, op=opx), r, w)

    def recip(o, i, r, w):
        op("dve", lambda e: e.reciprocal(out=o, in_=i), r, w)

    def dma(eng, o, i, r, w, dsem, indep=False):
        op(eng, lambda e: e.dma_start(out=o, in_=i), r, w, dsem, indep)

    X = ar.alloc([NCH, D], F32)
    bX = [Buf(f"X{c}") for c in range(NCH)]
    ident_f = ar.alloc([128], F32)
    ident_b = ar.alloc([128], BF16)
    cos_t = ar.alloc([NCH, 32], F32)
    sin_t = ar.alloc([NCH, 32], F32)
    cvec = ar.alloc([2112], F32)
    gain_na = cvec[:, 0:512]
    gain_dil = cvec[:, 512:1024]
    gout = [cvec[:, 1024:1536], cvec[:, 1536:2048]]
    brow = cvec[:, 2048:2084]
    namask = ar.alloc([NT_NA, 128], F32)
    Wr = ar.alloc([8, 36], F32)
    Lg = ar.alloc([NCH, 36], F32)
    gates = ar.alloc([NCH, 32], F32)
    ssq = ar.alloc([NCH], F32)
    rstd = ar.alloc([NCH], F32)
    bC = Buf("consts")
    bSsq, bRstd, bL, bGates, bRt = Buf("ssq"), Buf("rstd"), Buf("L"), Buf("gates"), Buf("rt")
    persist_top = ar.top

    sC = P.dma_sem("const", group=True)
    dma("sp", ident_f, c_ident[:, :], [], [bC], sC, True)
    dma("sp", cos_t, c_cos.rearrange("(c p) j -> p c j", p=128), [], [bC], sC, True)
    dma("sp", sin_t, c_sin.rearrange("(c p) j -> p c j", p=128), [], [bC], sC, True)
    dma("sp", namask, c_na.rearrange("p (t f) -> p t f", f=128), [], [bC], sC, True)
    sC2 = P.dma_sem("dilmask")
    dma("sp", cvec, c_vec[0:1, :].to_broadcast([128, 2112]), [], [bC], sC, True)
    dma("sp", Wr, c_wr.rearrange("p (k j) -> p k j", j=36), [], [bC], sC, True)
    bC2 = Buf("consts2")
    cp("dve", ident_b, ident_f, [bC], [bC2])
    ts("dve", gain_na[:, 0:256], gain_na[:, 0:256], 0.125, None, ALU.mult, None, [bC], [bC2])
    ts("dve", gain_dil[:, 0:256], gain_dil[:, 0:256], 0.125, None, ALU.mult, None, [bC], [bC2])
    CONST = [bC, bC2]

    sX = [P.dma_sem(f"x{q}") for q in range(4)]
    sG = P.dma_sem("gbc")
    sW = [P.dma_sem("wqkv0"), P.dma_sem("wqkv1")]
    sB = P.dma_sem("biasg")
    sE = [[P.dma_sem(f"wexp{i}_{m}") for m in range(3)] for i in range(2)]
    sPl = P.dma_sem("pl")
    sWp = P.dma_sem("wple")
    sO = [P.dma_sem("out0"), P.dma_sem("out1")]
    sD = P.dma_sem("dbg")

    def phase_begin():
        P.barrier()
        ar.top = persist_top

    def interleave(lists):
        n = max(len(l) for l in lists)
        for j in range(n):
            for l in lists:
                if j < len(l):
                    l[j]()

    def load_x(s, q):
        xv = x[s].rearrange("(c p) d -> p c d", p=128)
        dma("sp", X[:, 4 * q:4 * q + 4, :], xv[:, 4 * q:4 * q + 4, :], [],
            [bX[c] for c in range(4 * q, 4 * q + 4)], sX[q])

    def norm_phase(g_dram, hT, bHT, fp32_router):
        g_bc = ar.alloc([D], F32)
        bG = Buf("gbc")
        dma("sp", g_bc, g_dram[0:1, :].to_broadcast([128, D]), [], [bG], sG)
        bJ = Buf("junk")
        hdt = F32 if fp32_router else BF16
        NB = 3 if fp32_router else 4
        htmp = [ar.alloc([D], hdt) for _ in range(NB)]
        junk = htmp[0].bitcast(BF16)[:, 0:D] if fp32_router else htmp[0]
        bH = [Buf() for _ in range(NB)]
        if fp32_router:
            hTf = [ar.alloc([8, 128], F32) for _ in range(NB)]
            bHf = [Buf() for _ in range(NB)]
        for c in range(NCH):
            act(junk, X[:, c, :], AF.Square, [bX[c]], [bJ, bSsq], accum_out=ssq[:, c:c + 1])
        act(rstd, ssq, AF.Sqrt, [bSsq], [bRstd], scale=1.0 / D, bias=EPS)
        recip(rstd, rstd, [bRstd], [bRstd])
        psT = [ps[4 + j][:, :].bitcast(BF16) for j in range(4)]

        def stage_a(c):
            i = c % NB
            stt(htmp[i], X[:, c, :], rstd[:, c:c + 1], g_bc, ALU.mult, ALU.mult, [bX[c], bRstd, bG], [bH[i]])

        def stage_b(c):
            i = c % NB
            hb = htmp[i]
            if not fp32_router:
                pt = psT[i]
                bp = bPS[4 + i]
                for k in range(8):
                    tr(pt[:, k * 128:(k + 1) * 128], hb[:, k * 128:(k + 1) * 128], ident_b, [bH[i]] + CONST, [bp])
                cp("act", hT[:, :, c * 128:(c + 1) * 128], pt.rearrange("p (k t) -> p k t", k=8), [bp], [bHT[c]])
            else:
                b0 = 2 * i
                for k in range(8):
                    pb = ps[b0 + k // 4]
                    tr(pb[:, (k % 4) * 128:(k % 4 + 1) * 128], hb[:, k * 128:(k + 1) * 128], ident_f,
                       [bH[i]] + CONST, [bPS[b0 + k // 4]])
                hf = hTf[i]
                for half in range(2):
                    pb = ps[b0 + half]
                    cp("dve", hf[:, 4 * half:4 * half + 4, :], pb.rearrange("p (k t) -> p k t", k=4),
                       [bPS[b0 + half]], [bHf[i]])
                    cp("act", hT[:, 4 * half:4 * half + 4, c * 128:(c + 1) * 128],
                       hf[:, 4 * half:4 * half + 4, :], [bHf[i]], [bHT[c]])
                for k in range(8):
                    mm(ps[6][:, 0:36], hf[:, k, :], Wr[:, k, :], k == 0, k == 7, [bHf[i]] + CONST, [bPS[6]])
                cp("dve", Lg[:, c, :], ps[6][:, 0:36], [bPS[6]], [bL])

        LA = NB - 1
        for c in range(NCH + LA):
            if c < NCH:
                stage_a(c)
            if c >= LA:
                stage_b(c - LA)

    def router_math():
        rtmp = ar.alloc([10, NCH, 8], F32)
        R = [rtmp[:, i, :, :] for i in range(10)]
        rd = [bL, bRt] + CONST
        wr = [bRt]
        L = Lg

        def b3(ap2, n):
            return ap2.unsqueeze(2).to_broadcast([128, NCH, n])

        tt("dve", L, L, brow.unsqueeze(1).to_broadcast([128, NCH, 36]), ALU.add, rd, [bL])
        gl = L[:, :, 0:4]
        gmax = R[0][:, :, 0]
        red(gmax, gl, ALU.max, rd, wr)
        goh = R[1][:, :, 0:4]
        tt("dve", goh, gl, b3(gmax, 4), ALU.is_equal, rd, wr)
        gsh = R[2][:, :, 0:4]
        tt("dve", gsh, gl, b3(gmax, 4), ALU.subtract, rd, wr)
        act(gsh, gsh, AF.Exp, rd, wr)
        gsum = R[0][:, :, 1]
        red(gsum, gsh, ALU.add, rd, wr)
        gw = R[0][:, :, 2]
        recip(gw, gsum, rd, wr)
        el = R[3]
        tmp = R[4]
        for g in range(4):
            src = L[:, :, 4 + 8 * g:12 + 8 * g]
            if g == 0:
                tt("dve", el, src, b3(goh[:, :, 0], 8), ALU.mult, rd, wr)
            else:
                tt("dve", tmp, src, b3(goh[:, :, g], 8), ALU.mult, rd, wr)
                tt("dve", el, el, tmp, ALU.add, rd, wr)
        m1 = R[0][:, :, 3]
        red(m1, el, ALU.max, rd, wr)
        oh1 = R[5]
        tt("dve", oh1, el, b3(m1, 8), ALU.is_equal, rd, wr)
        el2 = R[6]
        ts("dve", tmp, oh1, -1e30, None, ALU.mult, None, rd, wr)
        tt("dve", el2, tmp, el, ALU.add, rd, wr)
        m2 = R[0][:, :, 4]
        red(m2, el2, ALU.max, rd, wr)
        oh2 = R[7]
        tt("dve", oh2, el2, b3(m2, 8), ALU.is_equal, rd, wr)
        dm = R[0][:, :, 5]
        tt("dve", dm, m2, m1, ALU.subtract, rd, wr)
        e21 = R[0][:, :, 6]
        act(e21, dm, AF.Exp, rd, wr)
        den = R[0][:, :, 7]
        ts("dve", den, e21, 1.0, None, ALU.add, None, rd, wr)
        w1 = R[8][:, :, 0]
        recip(w1, den, rd, wr)
        tt("dve", w1, w1, gw, ALU.mult, rd, wr)
        w2 = R[8][:, :, 1]
        tt("dve", w2, e21, w1, ALU.mult, rd, wr)
        g8 = R[9]
        tt("dve", g8, oh1, b3(w1, 8), ALU.mult, rd, wr)
        tt("dve", tmp, oh2, b3(w2, 8), ALU.mult, rd, wr)
        tt("dve", g8, g8, tmp, ALU.add, rd, wr)
        for g in range(4):
            tt("dve", gates[:, :, 8 * g:8 * g + 8], g8, b3(goh[:, :, g], 8), ALU.mult, rd, [bGates])

    def wt_views(raw):
        return raw.rearrange("p (k n) -> p k n", k=8), raw[:, 0:4096].rearrange("p (k n) -> p k n", k=4)

    def load_qkv_w(qp, raw, bW, sem, indep=False):
        group = qp // 2
        hbase = 8 * group + 4 * (qp % 2)
        Wt = wt_views(raw)[0]
        wv = w_qkv.rearrange("(k p) n -> p k n", p=128)
        for part in range(3):
            col = part * D + hbase * 64
            dma("pool", Wt[:, :, part * 256:(part + 1) * 256], wv[:, :, col:col + 256], [], [bW], sem, indep)

    def load_wo(group, raw, bW, sem):
        Wo = wt_views(raw)[1]
        dma("pool", Wo, w_o[group * 512:(group + 1) * 512, :].rearrange("(k p) n -> p k n", p=128), [], [bW], sem)

    def attn_pass(qp, hT, bHT, outg, bOut, Wraw, bWt):
        group = qp // 2
        hbase = 8 * group + 4 * (qp % 2)
        is_na = group == 0
        QKT = ar.alloc([4, S], BF16)
        QT = QKT[:, 0:2, :]
        KT = QKT[:, 2:4, :]
        Vx = ar.alloc([NCH, 4, 65], BF16)
        mark_t = ar.top
        Wt = wt_views(Wraw[qp % 2])[0]
        bW = bWt[qp % 2]
        bQT = [Buf(f"qt{c}") for c in range(NCH)]
        bKT = [Buf(f"kt{c}") for c in range(NCH)]
        bV = [Buf(f"v{c}") for c in range(NCH)]
        op("pool", lambda e: e.memset(Vx[:, :, :, 64:65], 1.0), [], bV)
        NQ = 4
        sq = [ar.alloc([512], F32) for _ in range(NQ)]
        qb = [ar.alloc([512], BF16) for _ in range(NQ)]
        s8 = [ar.alloc([8], F32) for _ in range(NQ)]
        bSq, bQb, bS8 = ([Buf() for _ in range(NQ)] for _ in range(3))
        if not is_na:
            rt_ = [ar.alloc([2, 8, 32], F32) for _ in range(NQ)]
            bRp = [Buf() for _ in range(NQ)]
        gain = gain_na if is_na else gain_dil
        psT = [ps[6][:, :].bitcast(BF16), ps[7][:, :].bitcast(BF16)]

        def slot(c):
            i = c % NQ
            pqk = ps[i]
            vb = 4 + i // 2
            pv = ps[vb][:, (i % 2) * 256:(i % 2 + 1) * 256]
            return i, pqk, pv, vb

        def qkv_mm(c):
            i, pqk, pv, vb = slot(c)
            for part in range(3):
                o = pqk[:, part * 256:(part + 1) * 256] if part < 2 else pv
                bo = bPS[i] if part < 2 else bPS[vb]
                for k in range(8):
                    mm(o, hT[:, k, c * 128:(c + 1) * 128], Wt[:, k, part * 256:(part + 1) * 256],
                       k == 0, k == 7, [bHT[c], bW], [bo])

        def post_steps(c):
            i, pqk, pv, vb = slot(c)
            q3 = sq[i].rearrange("p (h d) -> p h d", h=8)
            st = []
            st.append(lambda: act(sq[i], pqk, AF.Square, [bPS[i]], [bSq[i]]))
            st.append(lambda: cp("act", Vx[:, c, :, 0:64], pv.rearrange("p (h d) -> p h d", h=4),
                                 [bPS[vb]], [bV[c]]))
            st.append(lambda: red(s8[i], q3, ALU.add, [bSq[i]], [bS8[i]]))
            st.append(lambda: act(s8[i], s8[i], AF.Sqrt, [bS8[i]], [bS8[i]], scale=1.0 / 64, bias=EPS))
            st.append(lambda: recip(s8[i], s8[i], [bS8[i]], [bS8[i]]))
            st.append(lambda: tt("dve", q3, pqk.rearrange("p (h d) -> p h d", h=8),
                                 s8[i].unsqueeze(2).to_broadcast([128, 8, 64]), ALU.mult,
                                 [bPS[i], bS8[i]], [bSq[i]]))
            if is_na:
                st.append(lambda: tt("dve", qb[i], sq[i], gain, ALU.mult, [bSq[i]] + CONST, [bQb[i]]))
            else:
                st.append(lambda: tt("dve", sq[i], sq[i], gain, ALU.mult, [bSq[i]] + CONST, [bSq[i]]))
                v = sq[i].rearrange("p (h t j) -> p h t j", h=8, t=2)
                o = qb[i].rearrange("p (h t j) -> p h t j", h=8, t=2)
                x1, x2 = v[:, :, 0, :], v[:, :, 1, :]
                cb = cos_t[:, c, :].unsqueeze(1).to_broadcast([128, 8, 32])
                sb = sin_t[:, c, :].unsqueeze(1).to_broadcast([128, 8, 32])
                T = rt_[i]
                st.append(lambda: tt("dve", T[:, 0], x1, cb, ALU.mult, [bSq[i]] + CONST, [bRp[i]]))
                st.append(lambda: tt("dve", T[:, 1], x2, sb, ALU.mult, [bSq[i]] + CONST, [bRp[i]]))
                st.append(lambda: tt("dve", o[:, :, 0, :], T[:, 0], T[:, 1], ALU.subtract, [bRp[i]], [bQb[i]]))
                st.append(lambda: tt("dve", T[:, 0], x1, sb, ALU.mult, [bSq[i]] + CONST, [bRp[i]]))
                st.append(lambda: tt("dve", T[:, 1], x2, cb, ALU.mult, [bSq[i]] + CONST, [bRp[i]]))
                st.append(lambda: tt("dve", o[:, :, 1, :], T[:, 0], T[:, 1], ALU.add, [bRp[i]], [bQb[i]]))
            return st

        def qkv_tr(c):
            i = c % NQ
            j2 = c % 2
            pt = psT[j2]
            for j in range(4):
                tr(pt[:, j * 128:(j + 1) * 128], qb[i][:, j * 128:(j + 1) * 128], ident_b, [bQb[i]] + CONST, [bPS[6 + j2]])
            cp("act", QKT[:, :, c * 128:(c + 1) * 128], pt[:, 0:512].rearrange("p (a t) -> p a t", a=4),
               [bPS[6 + j2]], [bQT[c], bKT[c]])

        npair = NCH // 2
        for k in range(npair + 2):
            if k < npair:
                qkv_mm(2 * k)
                qkv_mm(2 * k + 1)
            if k >= 2:
                qkv_tr(2 * k - 4)
                qkv_tr(2 * k - 3)
            if k < npair:
                interleave([post_steps(2 * k), post_steps(2 * k + 1)])

        P.barrier()
        ar.top = mark_t
        NE = 6
        Ebuf = [ar.alloc([512], BF16) for _ in range(NE)]
        Pbuf = [ar.alloc([512], BF16) for _ in range(NE)]
        bE = [Buf() for _ in range(NE)]
        bPm = [Buf() for _ in range(NE)]
        rden = [ar.alloc([1], F32), ar.alloc([1], F32)]
        bRd = [Buf(), Buf()]
        SB = [0, 1, 2, 3, 6, 7]
        if not is_na:
            dilmask = ar.alloc([23, 128], BF16)
            bDm = Buf("dilmask")
            dma("pool", dilmask, c_dil.rearrange("p (t f) -> p t f", f=128), [], [bDm], sC2)
        if qp < 3:
            load_qkv_w(qp + 1, Wraw[(qp + 1) % 2], bWt[(qp + 1) % 2], sW[(qp + 1) % 2], True)
        if qp == 1:
            load_wo(0, Wraw[1], bWt[1], sW[1])
        if qp == 3:
            load_wo(1, Wraw[0], bWt[0], sW[0])
        if is_na:
            bias_t = ar.alloc([7, 128], F32)
            _eb = ar.alloc([NT_NA, 128], BF16)
            EB = [_eb, _eb]
            bBias = Buf()
            _beb = Buf()
            bEB = [_beb, _beb]
        items = []
        hq = 0
        for hl in range(4):
            for qc in range(NCH):
                if is_na:
                    kl = _NA_LISTS[qc]
                else:
                    kl = [(kc, kc - qc + 11) for kc in range(max(0, qc - 8), min(15, qc + 8) + 1)]
                ngr = (len(kl) + 3) // 4
                for gi in range(ngr):
                    items.append((hl, qc, kl[4 * gi:4 * gi + 4], gi == 0, gi == ngr - 1, hq))
                hq += 1

        def prep_head(hl):
            h8 = hbase - 8 * group + hl
            dma("sp", bias_t, biasG[h8].rearrange("p (t f) -> p t f", f=128), [], [bBias], sB)
            act(bias_t, bias_t, AF.Exp, [bBias], [bBias])
            e = EB[hl % 2]
            tt("dve", e[:, 0:7, :], bias_t, namask[:, 0:7, :], ALU.mult, [bBias] + CONST, [bEB[hl % 2]])
            for t in range(7, NT_NA):
                tt("dve", e[:, t, :], bias_t[:, _NA_TDELTA[t], :], namask[:, t, :], ALU.mult,
                   [bBias] + CONST, [bEB[hl % 2]])

        def emit_qk(idx):
            hl, qc, grp, first, last, hq_ = items[idx]
            if is_na and first and qc == 0:
                prep_head(hl)
            pair, sub = hl // 2, hl % 2
            rows = slice(64 * sub, 64 * sub + 64)
            bank = SB[idx % len(SB)]
            ng = len(grp)
            for j, (kc, _) in enumerate(grp):
                mm(ps[bank][:, j * 128:(j + 1) * 128], KT[rows, pair, kc * 128:(kc + 1) * 128],
                   QT[rows, pair, qc * 128:(qc + 1) * 128], True, True, [bKT[kc], bQT[qc]], [bPS[bank]])
            E = Ebuf[idx % NE]
            act(E[:, 0:ng * 128], ps[bank][:, 0:ng * 128], AF.Exp, [bPS[bank]], [bE[idx % NE]])
            Pm = Pbuf[idx % NE]
            tids = [t for _, t in grp]
            for (s0, ln) in _runs(tids):
                t0 = tids[s0]
                if is_na:
                    m = EB[hl % 2][:, t0:t0 + ln, :]
                    rdm = [bEB[hl % 2]]
                else:
                    m = dilmask[:, t0:t0 + ln, :]
                    rdm = [bDm]
                tt("dve", Pm[:, s0 * 128:(s0 + ln) * 128].rearrange("p (t f) -> p t f", f=128),
                   E[:, s0 * 128:(s0 + ln) * 128].rearrange("p (t f) -> p t f", f=128), m, ALU.mult,
                   [bE[idx % NE]] + rdm, [bPm[idx % NE]])

        def emit_pv(idx):
            hl, qc, grp, first, last, hq_ = items[idx]
            ab = 4 + hq_ % 2
            ng = len(grp)
            Pm = Pbuf[idx % NE]
            for j, (kc, _) in enumerate(grp):
                mm(ps[ab][:, 0:65], Pm[:, j * 128:(j + 1) * 128], Vx[:, kc, hl, :],
                   first and j == 0, last and j == ng - 1, [bPm[idx % NE], bV[kc]], [bPS[ab]])
            if last:
                r = rden[hq_ % 2]
                recip(r, ps[ab][:, 64:65], [bPS[ab]], [bRd[hq_ % 2]])
                hcol = (hbase - 8 * group + hl) * 64
                ts("dve", outg[:, qc, hcol:hcol + 64], ps[ab][:, 0:64], r, None, ALU.mult, None,
                   [bPS[ab], bRd[hq_ % 2]], [bOut[qc]])

        BT = 3
        nb_ = (len(items) + BT - 1) // BT
        for k in range(nb_ + 1):
            if k < nb_:
                for i in range(BT * k, min(BT * k + BT, len(items))):
                    emit_qk(i)
            if k >= 1:
                for i in range(BT * (k - 1), min(BT * k, len(items))):
                    emit_pv(i)

    def wo_phase(group, outg, bOut, Wo, bWo):
        yT = ar.alloc([4, S], BF16)
        junk = ar.alloc([512], BF16)
        ytmp = [ar.alloc([512], BF16) for _ in range(4)]
        bYT = [Buf() for _ in range(NCH)]
        bJ = Buf()
        bY = [Buf() for _ in range(4)]
        for c in range(NCH):
            act(junk, outg[:, c, :], AF.Square, [bOut[c]], [bJ, bSsq], accum_out=ssq[:, c:c + 1])
        act(rstd, ssq, AF.Sqrt, [bSsq], [bRstd], scale=1.0 / 512, bias=EPS)
        recip(rstd, rstd, [bRstd], [bRstd])
        psT = [ps[4 + j][:, :].bitcast(BF16) for j in range(4)]

        def sa(c):
            i = c % 4
            stt(ytmp[i], outg[:, c, :], rstd[:, c:c + 1], gout[group], ALU.mult, ALU.mult,
                [bOut[c], bRstd] + CONST, [bY[i]])

        def sb_(c):
            i = c % 4
            for k in range(4):
                tr(psT[i][:, k * 128:(k + 1) * 128], ytmp[i][:, k * 128:(k + 1) * 128], ident_b,
                   [bY[i]] + CONST, [bPS[4 + i]])
            cp("act", yT[:, :, c * 128:(c + 1) * 128], psT[i][:, 0:512].rearrange("p (k t) -> p k t", k=4),
               [bPS[4 + i]], [bYT[c]])

        def sc_(c):
            for db in range(2):
                b = (2 * c + db) % 4
                for k in range(4):
                    mm(ps[b][:, :], yT[:, k, c * 128:(c + 1) * 128], Wo[:, k, db * 512:(db + 1) * 512],
                       k == 0, k == 3, [bYT[c], bWo], [bPS[b]])
                xs = X[:, c, db * 512:(db + 1) * 512]
                tt("dve", xs, ps[b][:, :], xs, ALU.add, [bPS[b], bX[c]], [bX[c]])

        for c in range(NCH + 5):
            if c < NCH:
                sa(c)
            if 3 <= c < NCH + 3:
                sb_(c - 3)
            if c >= 5:
                sc_(c - 5)

    def moe_alloc():
        mb = dict(
            Wg=[ar.alloc([8, 512], BF16) for _ in range(2)],
            Wu=[ar.alloc([8, 512], BF16) for _ in range(2)],
            Wd=[ar.alloc([4, D], BF16) for _ in range(2)],
            bWg=[Buf(), Buf()], bWu=[Buf(), Buf()], bWd=[Buf(), Buf()],
        )
        return mb

    def moe_load(mb, e):
        s = e % 2
        dma("pool", mb["Wg"][s], w_eg[e].rearrange("(k p) f -> p k f", p=128), [], [mb["bWg"][s]], sE[s][0])
        dma("pool", mb["Wu"][s], w_eu[e].rearrange("(k p) f -> p k f", p=128), [], [mb["bWu"][s]], sE[s][1])
        dma("pool", mb["Wd"][s], w_ed[e].rearrange("(f p) d -> p f d", p=128), [], [mb["bWd"][s]], sE[s][2])

    def moe_phase(hT, bHT, mb):
        Wg, Wu, Wd = mb["Wg"], mb["Wu"], mb["Wd"]
        aT = ar.alloc([4, S], BF16)
        sl = [ar.alloc([512], BF16), ar.alloc([512], BF16)]
        bWg, bWu, bWd = mb["bWg"], mb["bWu"], mb["bWd"]
        bA = [[Buf() for _ in range(4)] for _ in range(4)]
        bSl = [Buf(), Buf()]
        cnt = {"gu": 0, "dn": 0}

        def gu(e, tb):
            s = e % 2
            for f in range(4):
                n = cnt["gu"]
                cnt["gu"] += 1
                pg, pu = ps[n % 2], ps[2 + n % 2]
                rd = [bHT[4 * tb + i] for i in range(4)]
                for k in range(8):
                    mm(pg[:, :], Wg[s][:, k, f * 128:(f + 1) * 128], hT[:, k, tb * 512:(tb + 1) * 512],
                       k == 0, k == 7, rd + [bWg[s]], [bPS[n % 2]])
                for k in range(8):
                    mm(pu[:, :], Wu[s][:, k, f * 128:(f + 1) * 128], hT[:, k, tb * 512:(tb + 1) * 512],
                       k == 0, k == 7, rd + [bWu[s]], [bPS[2 + n % 2]])
                act(sl[n % 2], pg[:, :], AF.Silu, [bPS[n % 2]], [bSl[n % 2]])
                tt("dve", aT[:, f, tb * 512:(tb + 1) * 512], pu[:, :], sl[n % 2], ALU.mult,
                   [bPS[2 + n % 2], bSl[n % 2]], [bA[f][tb]])

        def down(e, tb):
            s = e % 2
            for cc in range(4):
                c = 4 * tb + cc
                for db in range(2):
                    n = cnt["dn"]
                    cnt["dn"] += 1
                    b = 4 + n % 2
                    for f in range(4):
                        mm(ps[b][:, :], aT[:, f, c * 128:(c + 1) * 128], Wd[s][:, f, db * 512:(db + 1) * 512],
                           f == 0, f == 3, [bA[f][tb], bWd[s]], [bPS[b]])
                    xs = X[:, c, db * 512:(db + 1) * 512]
                    stt(xs, ps[b][:, :], gates[:, c, e:e + 1], xs, ALU.mult, ALU.add,
                        [bPS[b], bGates, bX[c]], [bX[c]])

        tasks = []
        for e in range(n_experts):
            for tb in range(4):
                tasks.append((e, tb))
        for i in range(len(tasks) + 1):
            if i < len(tasks):
                gu(*tasks[i])
            if i >= 1:
                e, tb = tasks[i - 1]
                down(e, tb)
                if tb == 3 and e + 2 < n_experts:
                    moe_load(mb, e + 2)

    def tail_alloc(s):
        tb = dict(pl=ar.alloc([NCH, 256], F32), pT=ar.alloc([2, S], BF16), Wpg=ar.alloc([8, D], BF16),
                  Wpp=ar.alloc([2, D], BF16), bPl=Buf(), bW=Buf())
        dma("sp", tb["pl"], pin[s].rearrange("(c p) j -> p c j", p=128), [], [tb["bPl"]], sPl)
        dma("pool", tb["Wpg"], w_pg.rearrange("(k p) n -> p k n", p=128), [], [tb["bW"]], sWp)
        dma("pool", tb["Wpp"], w_pp.rearrange("(k p) n -> p k n", p=128), [], [tb["bW"]], sWp)
        return tb

    def tail_phase(s, hT, bHT, tb):
        pl, pT, Wpg, Wpp, bPl, bW = tb["pl"], tb["pT"], tb["Wpg"], tb["Wpp"], tb["bPl"], tb["bW"]
        sg = [ar.alloc([512], F32), ar.alloc([512], F32)]
        ost = [ar.alloc([D], F32), ar.alloc([D], F32)]
        bPT = [Buf() for _ in range(NCH)]
        bSg = [Buf(), Buf()]
        bOs = [Buf(), Buf()]
        for c in range(NCH):
            b = 6 + c % 2
            for k in range(2):
                tr(ps[b][:, k * 128:(k + 1) * 128], pl[:, c, k * 128:(k + 1) * 128], ident_f, [bPl] + CONST, [bPS[b]])
            cp("act", pT[:, :, c * 128:(c + 1) * 128], ps[b][:, 0:256].rearrange("p (k t) -> p k t", k=2),
               [bPS[b]], [bPT[c]])
        n = 0
        for c in range(NCH):
            o = ost[c % 2]
            for db in range(2):
                i = n % 2
                n += 1
                pg, pp = ps[i], ps[2 + i]
                for k in range(8):
                    mm(pg[:, :], hT[:, k, c * 128:(c + 1) * 128], Wpg[:, k, db * 512:(db + 1) * 512],
                       k == 0, k == 7, [bHT[c], bW], [bPS[i]])
                for k in range(2):
                    mm(pp[:, :], pT[:, k, c * 128:(c + 1) * 128], Wpp[:, k, db * 512:(db + 1) * 512],
                       k == 0, k == 1, [bPT[c], bW], [bPS[2 + i]])
                act(sg[i], pg[:, :], AF.Sigmoid, [bPS[i]], [bSg[i]])
                tt("dve", sg[i], pp[:, :], sg[i], ALU.mult, [bPS[2 + i], bSg[i]], [bSg[i]])
                tt("dve", o[:, db * 512:(db + 1) * 512], sg[i], X[:, c, db * 512:(db + 1) * 512], ALU.add,
                   [bSg[i], bX[c]], [bOs[c % 2]])
            dma("sp", out[s, c * 128:(c + 1) * 128, :], o, [bOs[c % 2]], [Buf()], sO[c % 2])
            if c % 4 == 3 and s + 1 < nseq:
                load_x(s + 1, c // 4)

    def dump_x(dst, s):
        P.barrier()
        bd = Buf()
        for q in range(4):
            dma("sp", dst[s].rearrange("(c p) d -> p c d", p=128)[:, 4 * q:4 * q + 4, :], X[:, 4 * q:4 * q + 4, :],
                [bX[c] for c in range(4 * q, 4 * q + 4)], [bd], sD)
        P.barrier()

    class _Stop(Exception):
        pass

    nph = [0]

    def tick():
        nph[0] += 1
        if stop_after is not None and nph[0] >= stop_after:
            raise _Stop()

    try:
        for s in range(nseq):
            if s == 0:
                for q in range(4):
                    load_x(0, q)
            tick()
            phase_begin()
            hT = ar.alloc([8, S], BF16)
            bHT = [Buf(f"hT{c}") for c in range(NCH)]
            Wraw = [ar.alloc([6144], BF16), ar.alloc([6144], BF16)]
            bWt = [Buf("wt0"), Buf("wt1")]
            load_qkv_w(0, Wraw[0], bWt[0], sW[0])
            mark_h = ar.top
            norm_phase(g_attn, hT, bHT, False)
            tick()
            for group in range(2):
                P.barrier()
                ar.top = mark_h
                outg = ar.alloc([NCH, 512], BF16)
                bOut = [Buf(f"og{c}") for c in range(NCH)]
                mark_o = ar.top
                for sub in range(2):
                    P.barrier()
                    ar.top = mark_o
                    attn_pass(2 * group + sub, hT, bHT, outg, bOut, Wraw, bWt)
                    tick()
                P.barrier()
                ar.top = mark_o
                wslot = 1 if group == 0 else 0
                wo_phase(group, outg, bOut, wt_views(Wraw[wslot])[1], bWt[wslot])
                tick()
            if dbg:
                dump_x(dbg1, s)
            phase_begin()
            hT = ar.alloc([8, S], BF16)
            bHT = [Buf(f"h2T{c}") for c in range(NCH)]
            mb = moe_alloc()
            moe_load(mb, 0)
            if n_experts > 1:
                moe_load(mb, 1)
            mark_h = ar.top
            norm_phase(g_ffn, hT, bHT, True)
            tick()
            router_math()
            tick()
            P.barrier()
            ar.top = mark_h
            moe_phase(hT, bHT, mb)
            tick()
            if dbg:
                dump_x(dbg2, s)
            phase_begin()
            hT = ar.alloc([8, S], BF16)
            bHT = [Buf(f"h3T{c}") for c in range(NCH)]
            tbufs = tail_alloc(s)
            mark_h = ar.top
            norm_phase(g_ple, hT, bHT, False)
            tick()
            P.barrier()
            ar.top = mark_h
            tail_phase(s, hT, bHT, tbufs)
    except _Stop:
        P.barrier()
        for q in range(4):
            dma("sp", out[0].rearrange("(c p) d -> p c d", p=128)[:, 4 * q:4 * q + 4, :], X[:, 4 * q:4 * q + 4, :],
                [bX[c] for c in range(4 * q, 4 * q + 4)], [Buf()], sO[q % 2])

    fw = list(sO) + ([sD] if dbg else [])
    P.emit(stack, final_wait=fw)
    build_program.info = dict(ninst=len(P.insts), nwait=P.nwait, nsem=P.nsem, sbuf_peak=ar.peak)
    return nc


def _host_inputs(inp, nseq_per_core, ncores):
    f32 = lambda a: np.ascontiguousarray(np.asarray(a, dtype=np.float32))
    cos, sin = _rope_tables()
    ri, ci = _na_bias_gather_index()
    rpb = np.asarray(inp["rpb_na"], dtype=np.float32)[0]
    biasg = rpb[:, ri, ci].reshape(8, 128, 7 * 128)
    cvec = np.zeros((1, 2112), np.float32)
    cvec[0, 0:256] = np.tile(f32(inp["q_norm_na"]).reshape(64), 4)
    cvec[0, 256:512] = np.tile(f32(inp["k_norm_na"]).reshape(64), 4)
    cvec[0, 512:768] = np.tile(f32(inp["q_norm_dil"]).reshape(64), 4)
    cvec[0, 768:1024] = np.tile(f32(inp["k_norm_dil"]).reshape(64), 4)
    cvec[0, 1024:1536] = f32(inp["g_out_na"]).reshape(512)
    cvec[0, 1536:2048] = f32(inp["g_out_dil"]).reshape(512)
    cvec[0, 2048:2052] = f32(inp["b_router_group"]).reshape(4)
    cvec[0, 2052:2084] = f32(inp["b_router_expert"]).reshape(32)
    wcat = np.concatenate([f32(inp["w_router_group"][0])] + [f32(inp["w_router_expert"][0][g]) for g in range(4)],
                          axis=1)
    wr = np.ascontiguousarray(wcat.reshape(8, 128, 36).transpose(1, 0, 2).reshape(128, 8 * 36))
    shared = {
        "g_attn": f32(inp["g_attn"]).reshape(1, D),
        "w_qkv": f32(inp["w_qkv"][0]),
        "c_vec": cvec,
        "c_wr": wr,
        "biasG": f32(biasg),
        "w_o": f32(inp["w_o"][0]),
        "g_ffn": f32(inp["g_ffn"]).reshape(1, D),
        "w_eg": f32(inp["w_exp_gate"][0]),
        "w_eu": f32(inp["w_exp_up"][0]),
        "w_ed": f32(inp["w_exp_down"][0]),
        "g_ple": f32(inp["g_ple"]).reshape(1, D),
        "w_pg": f32(inp["w_ple_gate"][0]),
        "w_pp": f32(inp["w_ple_proj"][0]),
        "c_ident": np.eye(128, dtype=np.float32),
        "c_cos": cos,
        "c_sin": sin,
        "c_dil": _dil_mask().reshape(128, 23 * 128),
        "c_na": np.ascontiguousarray(_NA_TILES.reshape(128, NT_NA * 128)),
    }
    xs = f32(inp["x"])
    ps_ = f32(inp["p"][0])
    maps = []
    for i in range(ncores):
        m = dict(shared)
        m["x"] = xs[i * nseq_per_core:(i + 1) * nseq_per_core]
        m["p"] = ps_[i * nseq_per_core:(i + 1) * nseq_per_core]
        maps.append(m)
    return maps


def kernel(**inputs):
    B = np.asarray(inputs["x"]).shape[0]
    nseq = B // NCORES
    nc = build_program(nseq=nseq)
    maps = _host_inputs(inputs, nseq, NCORES)
    res = run_bass_kernel_spmd(nc, maps, core_ids=list(range(NCORES)))
    outs = [np.asarray(r["out"], dtype=np.float32) for r in res.results]
    return np.concatenate(outs, axis=0)
```
